# Optimizing a Trainium2 kernel written in Bass

```python
import math
import jax
import jax.numpy as jnp
from jax import lax
import numpy as np

D_MODEL = 2048
BATCH = 4
SEQ = 4096
DEPTH = 2

GRID_W = 64
CTX_LEN = 256
HEAD_DIM = 128
AXIS_DIM = HEAD_DIM // 2
ROPE_THETA = 10000.0
Q_BLOCK = 128
A_HEADS = 8
A_KV_HEADS = 2
B_HEADS = 8
B_KV_HEADS = 2
WINDOW = 128
C_QK_HEADS = 4
C_V_HEADS = 8
CONV_K = 5
CHUNK = 64
N_EXPERTS = 64
TOP_K = 8
EXPERT_FF = 512
SHARED_FF = 512
ROUTED_SCALE = 2.5
MOE_BLOCK = 128
MOD_WIDTH = 6 * D_MODEL
DEEPNORM_ALPHA = (2.0 * DEPTH) ** 0.25
DEEPNORM_BETA = (8.0 * DEPTH) ** -0.25
LN_EPS = 1e-5
RMS_EPS = 1e-6

A_Q = A_HEADS * HEAD_DIM
A_KV = A_KV_HEADS * HEAD_DIM
B_Q = B_HEADS * HEAD_DIM
B_KV = B_KV_HEADS * HEAD_DIM
C_QK = C_QK_HEADS * HEAD_DIM
C_V = C_V_HEADS * HEAD_DIM
IN_SPLITS = (A_Q, A_KV, A_KV, B_Q, B_KV, B_KV, C_QK, C_QK, C_V, C_V, 2 * C_V_HEADS, 2 * C_V_HEADS, 3 * D_MODEL)
IN_WIDTH = sum(IN_SPLITS)
IN_OFFSETS = tuple(int(o) for o in np.cumsum(IN_SPLITS)[:-1])

kernel_name = 'hybrid_dit_gqa_swa_gdn_moe'


def _layernorm(x):
    xf = x.astype(jnp.float32)
    mu = jnp.mean(xf, axis=-1, keepdims=True)
    var = jnp.mean(jnp.square(xf - mu), axis=-1, keepdims=True)
    return (xf - mu) * lax.rsqrt(var + LN_EPS)


def _post_norm(x, g, b):
    return (_layernorm(x) * g.astype(jnp.float32) + b.astype(jnp.float32)).astype(x.dtype)


def _rmsnorm(x, g):
    xf = x.astype(jnp.float32)
    y = xf * lax.rsqrt(jnp.mean(jnp.square(xf), axis=-1, keepdims=True) + RMS_EPS)
    return (y * g.astype(jnp.float32)).astype(x.dtype)


def _l2norm(x):
    return x * lax.rsqrt(jnp.sum(jnp.square(x), axis=-1, keepdims=True) + RMS_EPS)


def _axial_tables(n_tokens, dtype):
    rows = n_tokens // GRID_W
    row = jnp.repeat(jnp.arange(rows, dtype=jnp.float32), GRID_W)
    col = jnp.tile(jnp.arange(GRID_W, dtype=jnp.float32), rows)
    inv_freq = ROPE_THETA ** (-jnp.arange(0, AXIS_DIM, 2, dtype=jnp.float32) / AXIS_DIM)
    ang_r = row[:, None] * inv_freq[None, :]
    ang_c = col[:, None] * inv_freq[None, :]
    ang = jnp.concatenate([ang_r, ang_r, ang_c, ang_c], axis=-1)
    return jnp.cos(ang).astype(dtype), jnp.sin(ang).astype(dtype)


def _rotate_half(t):
    a, b = jnp.split(t, 2, axis=-1)
    return jnp.concatenate([-b, a], axis=-1)


def _apply_axial_rope(t, cos, sin):
    t_row, t_col = jnp.split(t, 2, axis=-1)
    rot = jnp.concatenate([_rotate_half(t_row), _rotate_half(t_col)], axis=-1)
    return t * cos[None, :, None, :] + rot * sin[None, :, None, :]


def _heads(t, n_heads):
    return t.reshape(t.shape[:2] + (n_heads, HEAD_DIM))


def _group(q, n_kv):
    b, s, h, d = q.shape
    return q.reshape(b, s, n_kv, h // n_kv, d)


def _global_attn_latent(q, k_all, v_all):
    b, s, hkv, g, d = q.shape
    nb = s // Q_BLOCK
    scale = d ** -0.5
    q_blocks = jnp.moveaxis(q.reshape(b, nb, Q_BLOCK, hkv, g, d), 1, 0)

    def one_block(qi):
        sc = jnp.einsum('bqhgd,bkhd->bhgqk', qi, k_all).astype(jnp.float32) * scale
        p = jax.nn.softmax(sc, axis=-1).astype(v_all.dtype)
        return jnp.einsum('bhgqk,bkhd->bqhgd', p, v_all)

    o = lax.map(one_block, q_blocks)
    return jnp.moveaxis(o, 0, 1).reshape(b, s, hkv * g * d)


def _ctx_attn(q, k, v, sink=None):
    b, s, hkv, g, d = q.shape
    sc = jnp.einsum('bqhgd,bkhd->bhgqk', q, k).astype(jnp.float32) * d ** -0.5
    if sink is not None:
        col = jnp.broadcast_to(sink.reshape(1, hkv, g, 1, 1).astype(jnp.float32), sc.shape[:-1] + (1,))
        p = jax.nn.softmax(jnp.concatenate([sc, col], axis=-1), axis=-1)[..., :-1]
    else:
        p = jax.nn.softmax(sc, axis=-1)
    return jnp.einsum('bhgqk,bkhd->bqhgd', p.astype(v.dtype), v).reshape(b, s, hkv * g * d)


def _window_attn_latent(q, k, v, k_ctx, v_ctx, sink):
    b, s, hkv, g, d = q.shape
    nb = s // Q_BLOCK
    span = 3 * Q_BLOCK
    scale = d ** -0.5

    def band(t):
        tb = jnp.pad(t, ((0, 0), (Q_BLOCK, Q_BLOCK), (0, 0), (0, 0))).reshape(b, nb + 2, Q_BLOCK, hkv, d)
        return jnp.concatenate([tb[:, :-2], tb[:, 1:-1], tb[:, 2:]], axis=2)

    k_band, v_band = band(k), band(v)
    qb = q.reshape(b, nb, Q_BLOCK, hkv, g, d)
    s_win = jnp.einsum('bnqhgd,bnkhd->bhgnqk', qb, k_band).astype(jnp.float32) * scale
    s_ctx = jnp.einsum('bnqhgd,bchd->bhgnqc', qb, k_ctx).astype(jnp.float32) * scale
    q_pos = jnp.arange(s).reshape(nb, Q_BLOCK)
    k_pos = (jnp.arange(nb) * Q_BLOCK - Q_BLOCK)[:, None] + jnp.arange(span)[None, :]
    kp = k_pos[:, None, :]
    valid = (jnp.abs(kp - q_pos[:, :, None]) <= WINDOW) & (kp >= 0) & (kp < s)
    s_win = jnp.where(valid, s_win, -jnp.inf)
    sink_col = jnp.broadcast_to(sink.reshape(1, hkv, g, 1, 1, 1).astype(jnp.float32), s_win.shape[:-1] + (1,))
    p = jax.nn.softmax(jnp.concatenate([s_win, s_ctx, sink_col], axis=-1), axis=-1).astype(v.dtype)
    o = (jnp.einsum('bhgnqk,bnkhd->bnqhgd', p[..., :span], v_band)
         + jnp.einsum('bhgnqc,bchd->bnqhgd', p[..., span:span + k_ctx.shape[1]], v_ctx))
    return o.reshape(b, s, hkv * g * d)


def _short_conv(t, w):
    y = lax.conv_general_dilated(t, w[:, None, :].astype(t.dtype), window_strides=(1,),
                                 padding=[(CONV_K // 2, CONV_K // 2)],
                                 dimension_numbers=('NWC', 'WIO', 'NWC'),
                                 feature_group_count=t.shape[-1])
    return jax.nn.silu(y)


def _delta_inputs(qkv, a, bgate, a_log, dt_bias):
    b, n, _ = qkv.shape
    q, k, v = jnp.split(qkv.astype(jnp.float32), (C_QK, 2 * C_QK), axis=-1)
    rep = C_V_HEADS // C_QK_HEADS
    q = jnp.repeat(_l2norm(_heads(q, C_QK_HEADS)), rep, axis=2) * HEAD_DIM ** -0.5
    k = jnp.repeat(_l2norm(_heads(k, C_QK_HEADS)), rep, axis=2)
    v = _heads(v, C_V_HEADS)
    a = a.astype(jnp.float32).reshape(b, n, 2, C_V_HEADS)
    g = -jnp.exp(a_log.astype(jnp.float32)) * jax.nn.softplus(a + dt_bias.astype(jnp.float32))
    beta = jax.nn.sigmoid(bgate.astype(jnp.float32).reshape(b, n, 2, C_V_HEADS))
    return q, k, v, g, beta


def _delta_scan(q, k, v, g, beta, state0):
    b, n_tok, h, dk = q.shape
    dv = v.shape[-1]
    n = n_tok // CHUNK

    def to_chunks(t):
        t = t.reshape((b, n, CHUNK) + t.shape[2:])
        return jnp.moveaxis(t, (1, 2), (0, 3))

    qc, kc, vc = to_chunks(q), to_chunks(k), to_chunks(v)
    gc = jnp.cumsum(to_chunks(g), axis=-1)
    bc = to_chunks(beta)
    idx = jnp.arange(CHUNK)
    lower = idx[:, None] >= idx[None, :]
    strict = idx[:, None] > idx[None, :]
    decay = jnp.exp(jnp.where(lower, gc[..., :, None] - gc[..., None, :], -jnp.inf))
    kb = kc * bc[..., None]
    a_mat = jnp.where(strict, jnp.einsum('nbhik,nbhjk->nbhij', kb, kc) * decay, 0.0)
    eye = jnp.eye(CHUNK, dtype=jnp.float32)
    t_inv = lax.linalg.triangular_solve(eye + a_mat, jnp.broadcast_to(eye, a_mat.shape), left_side=True, lower=True)
    u = t_inv @ (vc * bc[..., None])
    w = t_inv @ (kb * jnp.exp(gc)[..., None])
    qk = jnp.einsum('nbhik,nbhjk->nbhij', qc, kc) * decay
    q_dec = qc * jnp.exp(gc)[..., None]
    g_last = gc[..., -1]
    k_dec = kc * jnp.exp(g_last[..., None] - gc)[..., None]

    def step(state, xs):
        u_i, w_i, qd_i, qk_i, kd_i, gl_i = xs
        v_new = u_i - w_i @ state
        o_i = qd_i @ state + qk_i @ v_new
        state = state * jnp.exp(gl_i)[..., None, None] + jnp.einsum('bhck,bhcv->bhkv', kd_i, v_new)
        return state, o_i

    state_fin, o = lax.scan(step, state0, (u, w, q_dec, qk, k_dec, g_last))
    o = jnp.moveaxis(o, (0, 3), (1, 2)).reshape(b, n_tok, h, dv)
    return o, state_fin


def _bidir_delta(ctx_in, lat_in):
    qc, kc, vc, gc, bc = ctx_in
    ql, kl, vl, gl, bl = lat_in
    state0 = jnp.zeros((ql.shape[0], C_V_HEADS, HEAD_DIM, HEAD_DIM), jnp.float32)

    def flip(t):
        return jnp.flip(t, axis=1)

    o_cf, s_f = _delta_scan(qc, kc, vc, gc[:, :, 0], bc[:, :, 0], state0)
    o_lf, _ = _delta_scan(ql, kl, vl, gl[:, :, 0], bl[:, :, 0], s_f)
    o_cb, s_b = _delta_scan(flip(qc), flip(kc), flip(vc), flip(gc[:, :, 1]), flip(bc[:, :, 1]), state0)
    o_lb, _ = _delta_scan(flip(ql), flip(kl), flip(vl), flip(gl[:, :, 1]), flip(bl[:, :, 1]), s_b)
    return o_cf + flip(o_cb), o_lf + flip(o_lb)


def _token_mixer(h, hc, w_in, q_norm_a, k_norm_a, sink_b, conv_c, a_log_c, dt_bias_c, norm_c,
                 w_br_a, w_br_b, w_br_c, w_out, cos, sin, need_ctx):
    aq, ak, av, bq, bk, bv, cq, ck, cv, cz, ca, cb, gates = jnp.split(h @ w_in, IN_OFFSETS, axis=-1)
    aq_c, ak_c, av_c, bq_c, bk_c, bv_c, cq_c, ck_c, cv_c, cz_c, ca_c, cb_c, gates_c = jnp.split(hc @ w_in, IN_OFFSETS, axis=-1)

    def rope(t):
        return _apply_axial_rope(t, cos, sin)

    qa = rope(_rmsnorm(_heads(aq, A_HEADS), q_norm_a))
    ka = rope(_rmsnorm(_heads(ak, A_KV_HEADS), k_norm_a))
    ka_c = _rmsnorm(_heads(ak_c, A_KV_HEADS), k_norm_a)
    va, va_c = _heads(av, A_KV_HEADS), _heads(av_c, A_KV_HEADS)
    o_a = _global_attn_latent(_group(qa, A_KV_HEADS), jnp.concatenate([ka_c, ka], axis=1),
                              jnp.concatenate([va_c, va], axis=1))
    qb = rope(_heads(bq, B_HEADS))
    kb = rope(_heads(bk, B_KV_HEADS))
    kb_c, vb, vb_c = _heads(bk_c, B_KV_HEADS), _heads(bv, B_KV_HEADS), _heads(bv_c, B_KV_HEADS)
    o_b = _window_attn_latent(_group(qb, B_KV_HEADS), kb, vb, kb_c, vb_c, sink_b)
    lat_in = _delta_inputs(_short_conv(jnp.concatenate([cq, ck, cv], axis=-1), conv_c), ca, cb, a_log_c, dt_bias_c)
    ctx_in = _delta_inputs(_short_conv(jnp.concatenate([cq_c, ck_c, cv_c], axis=-1), conv_c), ca_c, cb_c, a_log_c, dt_bias_c)
    o_c_ctx, o_c = _bidir_delta(ctx_in, lat_in)

    def gated_out(o, z):
        zf = _heads(z, C_V_HEADS).astype(jnp.float32)
        return (_rmsnorm(o, norm_c) * jax.nn.silu(zf)).reshape(o.shape[:2] + (C_V,)).astype(z.dtype)

    def merge(o_a_, o_b_, o_c_, gates_):
        g_a, g_b, g_c = jnp.split(jax.nn.sigmoid(gates_), 3, axis=-1)
        y_ = g_a * (o_a_ @ w_br_a) + g_b * (o_b_ @ w_br_b) + g_c * (o_c_ @ w_br_c)
        return y_ @ w_out

    y = merge(o_a, o_b, gated_out(o_c, cz), gates)
    if not need_ctx:
        return y, None
    qa_c = _rmsnorm(_heads(aq_c, A_HEADS), q_norm_a)
    o_a_c = _ctx_attn(_group(qa_c, A_KV_HEADS), ka_c, va_c)
    o_b_c = _ctx_attn(_group(_heads(bq_c, B_HEADS), B_KV_HEADS), kb_c, vb_c, sink_b)
    y_c = merge(o_a_c, o_b_c, gated_out(o_c_ctx, cz_c), gates_c)
    return y, y_c


def _swiglu(t, w_gate, w_up, w_down):
    return (jax.nn.silu(t @ w_gate) * (t @ w_up)) @ w_down


def _moe(t, w_router, router_bias, w1, w3, w2, ws1, ws3, ws2):
    n_tok, d = t.shape
    scores = jax.nn.sigmoid((t @ w_router).astype(jnp.float32))
    _, idx = lax.top_k(scores + router_bias.astype(jnp.float32), TOP_K)
    gate = jnp.take_along_axis(scores, idx, axis=-1)
    gate = gate / jnp.sum(gate, axis=-1, keepdims=True) * ROUTED_SCALE
    n_assign = n_tok * TOP_K
    n_blocks = -(-n_assign // MOE_BLOCK) + N_EXPERTS
    e_flat = idx.reshape(-1)
    order = jnp.argsort(e_flat)
    e_sorted = e_flat[order]
    counts = jnp.bincount(e_flat, length=N_EXPERTS)
    padded = (counts + MOE_BLOCK - 1) // MOE_BLOCK * MOE_BLOCK
    padded_end = jnp.cumsum(padded)
    dest = (padded_end - padded)[e_sorted] + jnp.arange(n_assign) - (jnp.cumsum(counts) - counts)[e_sorted]
    slot_tok = jnp.full((n_blocks * MOE_BLOCK,), n_tok, jnp.int32).at[dest].set((order // TOP_K).astype(jnp.int32))
    slot_gate = jnp.zeros((n_blocks * MOE_BLOCK,), jnp.float32).at[dest].set(gate.reshape(-1)[order])
    block_expert = jnp.minimum(jnp.searchsorted(padded_end, jnp.arange(n_blocks) * MOE_BLOCK, side='right'), N_EXPERTS - 1)
    t_pad = jnp.concatenate([t, jnp.zeros((1, d), t.dtype)], axis=0)

    def one_block(args):
        tok, gt, e = args
        return _swiglu(t_pad[tok], w1[e], w3[e], w2[e]) * gt[:, None].astype(t.dtype)

    out = lax.map(one_block, (slot_tok.reshape(n_blocks, MOE_BLOCK), slot_gate.reshape(n_blocks, MOE_BLOCK), block_expert))
    routed = jnp.zeros((n_tok + 1, d), t.dtype).at[slot_tok].add(out.reshape(-1, d))[:n_tok]
    return routed + _swiglu(t, ws1, ws3, ws2)


def setup_inputs(seed: int = 0) -> dict:
    key = jax.random.key(seed)
    ks = jax.random.split(key, 30)
    f32 = jnp.float32
    L, D, E = DEPTH, D_MODEL, N_EXPERTS

    def nrm(k, shape, fan_in, gain=1.0):
        return jax.random.normal(k, shape, f32) * (gain * fan_in ** -0.5)

    def near_one(k, shape):
        return 1.0 + 0.05 * jax.random.normal(k, shape, f32)

    dt = jnp.exp(jax.random.uniform(ks[12], (L, 2, C_V_HEADS), f32) * (math.log(0.1) - math.log(0.001)) + math.log(0.001))
    return {
        'x': jax.random.normal(ks[0], (BATCH, SEQ, D), f32),
        'c': jax.random.normal(ks[1], (BATCH, D), f32),
        'ctx': jax.random.normal(ks[2], (BATCH, CTX_LEN, D), f32),
        'c_ctx': jax.random.normal(ks[3], (D,), f32),
        'w_mod': nrm(ks[4], (L, D, MOD_WIDTH), D, 0.5),
        'b_mod': 0.02 * jax.random.normal(ks[5], (L, MOD_WIDTH), f32),
        'w_in': nrm(ks[6], (L, D, IN_WIDTH), D),
        'q_norm_a': near_one(ks[7], (L, HEAD_DIM)),
        'k_norm_a': near_one(ks[8], (L, HEAD_DIM)),
        'sink_b': 0.5 * jax.random.normal(ks[9], (L, B_HEADS), f32),
        'conv_c': nrm(ks[10], (L, CONV_K, 2 * C_QK + C_V), CONV_K),
        'a_log_c': jnp.log(jax.random.uniform(ks[11], (L, 2, C_V_HEADS), f32, 1.0, 16.0)),
        'dt_bias_c': dt + jnp.log(-jnp.expm1(-dt)),
        'norm_c': near_one(ks[13], (L, HEAD_DIM)),
        'w_br_a': nrm(ks[14], (L, A_Q, D), A_Q),
        'w_br_b': nrm(ks[15], (L, B_Q, D), B_Q),
        'w_br_c': nrm(ks[16], (L, C_V, D), C_V),
        'w_out': nrm(ks[17], (L, D, D), D, DEEPNORM_BETA),
        'ln1_g': near_one(ks[18], (L, D)),
        'ln1_b': 0.02 * jax.random.normal(ks[19], (L, D), f32),
        'w_router': nrm(ks[20], (L, D, E), D),
        'router_bias': 0.01 * jax.random.normal(ks[21], (L, E), f32),
        'w1': nrm(ks[22], (L, E, D, EXPERT_FF), D),
        'w3': nrm(ks[23], (L, E, D, EXPERT_FF), D),
        'w2': nrm(ks[24], (L, E, EXPERT_FF, D), EXPERT_FF, DEEPNORM_BETA),
        'ws1': nrm(ks[25], (L, D, SHARED_FF), D),
        'ws3': nrm(ks[26], (L, D, SHARED_FF), D),
        'ws2': nrm(ks[27], (L, SHARED_FF, D), SHARED_FF, DEEPNORM_BETA),
        'ln2_g': near_one(ks[28], (L, D)),
        'ln2_b': 0.02 * jax.random.normal(ks[29], (L, D), f32),
    }


def reference(x, c, ctx, c_ctx, w_mod, b_mod, w_in, q_norm_a, k_norm_a, sink_b, conv_c, a_log_c, dt_bias_c,
              norm_c, w_br_a, w_br_b, w_br_c, w_out, ln1_g, ln1_b, w_router, router_bias, w1, w3, w2,
              ws1, ws3, ws2, ln2_g, ln2_b):
    b, s, d = x.shape
    n_ctx = ctx.shape[1]
    cos, sin = _axial_tables(s, x.dtype)
    x = _layernorm(x).astype(x.dtype)
    xc = _layernorm(ctx).astype(ctx.dtype)
    silu_c = jax.nn.silu(c)
    silu_cc = jax.nn.silu(c_ctx)
    for l in range(DEPTH):
        need_ctx = l < DEPTH - 1
        sh1, sc1, gt1, sh2, sc2, gt2 = jnp.split(silu_c @ w_mod[l] + b_mod[l], 6, axis=-1)
        sh1c, sc1c, gt1c, sh2c, sc2c, gt2c = jnp.split(silu_cc @ w_mod[l] + b_mod[l], 6, axis=-1)
        h = x * (1 + sc1[:, None]) + sh1[:, None]
        hc = xc * (1 + sc1c) + sh1c
        y, y_c = _token_mixer(h, hc, w_in[l], q_norm_a[l], k_norm_a[l], sink_b[l], conv_c[l], a_log_c[l],
                              dt_bias_c[l], norm_c[l], w_br_a[l], w_br_b[l], w_br_c[l], w_out[l], cos, sin, need_ctx)
        x = _post_norm(DEEPNORM_ALPHA * x + gt1[:, None] * y, ln1_g[l], ln1_b[l])
        h = x * (1 + sc2[:, None]) + sh2[:, None]
        if need_ctx:
            xc = _post_norm(DEEPNORM_ALPHA * xc + gt1c * y_c, ln1_g[l], ln1_b[l])
            hc = xc * (1 + sc2c) + sh2c
            f = _moe(jnp.concatenate([h.reshape(-1, d), hc.reshape(-1, d)], axis=0), w_router[l], router_bias[l],
                     w1[l], w3[l], w2[l], ws1[l], ws3[l], ws2[l])
            f_lat = f[:b * s].reshape(b, s, d)
            f_ctx = f[b * s:].reshape(b, n_ctx, d)
            xc = _post_norm(DEEPNORM_ALPHA * xc + gt2c * f_ctx, ln2_g[l], ln2_b[l])
        else:
            f_lat = _moe(h.reshape(-1, d), w_router[l], router_bias[l], w1[l], w3[l], w2[l],
                         ws1[l], ws3[l], ws2[l]).reshape(b, s, d)
        x = _post_norm(DEEPNORM_ALPHA * x + gt2[:, None] * f_lat, ln2_g[l], ln2_b[l])
    return x
```

```python
import os
import math
from contextlib import ExitStack

import numpy as np
import concourse.bass as bass
import concourse.mybir as mybir
from concourse.bass_utils import run_bass_kernel_spmd

F32 = mybir.dt.float32
BF16 = mybir.dt.bfloat16
I32 = mybir.dt.int32
U32 = mybir.dt.uint32
AF = mybir.ActivationFunctionType
ALU = mybir.AluOpType
AX = mybir.AxisListType

D = 2048
KC = 16
NCTX = 256
SEQ = 4096
T = NCTX + SEQ
NT = T // 128
DEPTH = 2
HD = 128
IN_W = 12320
MODW = 6 * D
NE = 64
TOPK = 8
EFF = 512
ALPHA = (2.0 * DEPTH) ** 0.25
LN_EPS = 1e-5
RMS_EPS = 1e-6
O_AQ, O_AK, O_AV = 0, 1024, 1280
O_BQ, O_BK, O_BV = 1536, 2560, 2816
O_CQ, O_CK, O_CV, O_CZ = 3072, 3584, 4096, 5120
O_CA, O_CB = 6144, 6160
O_G = 6176
NB = int(os.environ.get("MK_NB", "1"))
GRAN = 256
NBIG = (T * TOPK) // GRAN + NE
NBLK_R = NBIG * (GRAN // 128)
BASE_SH = NBLK_R * 128
NSLOT = BASE_SH + T


class Sched:
    def __init__(self, nc, stack, n_dma_sems=(8, 4, 8)):
        self.nc = nc
        self.eng = {"pe": nc.tensor, "dve": nc.vector, "act": nc.scalar,
                    "pool": nc.gpsimd, "sp": nc.sync}
        self.sem = {}
        self.cnt = {}
        for e in ("pe", "dve", "act", "pool"):
            self.sem[e] = stack.enter_context(nc.semaphore("s_" + e))
            self.cnt[e] = 0
        self.dsem = {}
        self.dcnt = {}
        for q, n in zip(("sp", "act", "pool"), n_dma_sems):
            self.dsem[q] = [stack.enter_context(nc.semaphore("d_%s%d" % (q, i))) for i in range(n)]
            self.dcnt[q] = 0
        self.waited = {}
        self.lastw = {}
        self.readers = {}
        self.n_inst = 0
        self.nbar = 0
        self.sem_arrive = stack.enter_context(nc.semaphore("s_arrive"))
        self.sem_release = stack.enter_context(nc.semaphore("s_release"))

    def _wait(self, e, ev):
        if ev is None:
            return
        sem, name, val, src = ev
        if src == e and e == "pe":
            return
        k = (e, name)
        if self.waited.get(k, 0) >= val:
            return
        self.eng[e].wait_ge(sem, val)
        self.waited[k] = val
        self.n_inst += 1

    def _deps(self, e, reads, writes):
        for k in reads:
            self._wait(e, self.lastw.get(k))
        for k in writes:
            self._wait(e, self.lastw.get(k))
            for ev in self.readers.get(k, ()):
                self._wait(e, ev)

    def _commit(self, ev, reads, writes):
        for k in writes:
            self.lastw[k] = ev
            self.readers[k] = []
        for k in reads:
            lst = self.readers.setdefault(k, [])
            lst[:] = [x for x in lst if x[1] != ev[1]]
            lst.append(ev)

    def op(self, e, fn, *args, reads=(), writes=(), **kw):
        self._deps(e, reads, writes)
        ins = fn(*args, **kw)
        self.cnt[e] += 1
        ins.then_inc(self.sem[e], 1)
        ev = (self.sem[e], "s_" + e, self.cnt[e], e)
        self._commit(ev, reads, writes)
        self.n_inst += 1
        return ev

    def dma(self, q, out, in_, reads=(), writes=(), indirect=None, **kw):
        sems = self.dsem[q]
        i = self.dcnt[q]
        r = i % len(sems)
        m = i // len(sems)
        sem = sems[r]
        name = "d_%s%d" % (q, r)
        if m > 0:
            k = (q, name)
            if self.waited.get(k, 0) < 16 * m:
                self.eng[q].wait_ge(sem, 16 * m)
                self.waited[k] = 16 * m
        self._deps(q, reads, writes)
        if indirect is None:
            ins = self.eng[q].dma_start(out=out, in_=in_, **kw)
        else:
            ins = indirect()
        ins.then_inc(sem, 16)
        self.dcnt[q] += 1
        ev = (sem, name, 16 * (m + 1), "dma_" + q)
        self._commit(ev, reads, writes)
        self.n_inst += 1
        return ev

    def barrier(self):
        evs = []
        for e in ("pe", "dve", "act", "pool"):
            if self.cnt[e] > 0:
                evs.append((self.sem[e], "s_" + e, self.cnt[e], e))
        for q in ("sp", "act", "pool"):
            n = len(self.dsem[q])
            for r in range(n):
                issued = (self.dcnt[q] - r + n - 1) // n
                if issued > 0:
                    evs.append((self.dsem[q][r], "d_%s%d" % (q, r), 16 * issued, "dma_" + q))
        engs = ("pe", "dve", "act", "pool", "sp")
        for e in engs:
            for ev in evs:
                sem, name, val, src = ev
                k = (e, name)
                if self.waited.get(k, 0) >= val:
                    continue
                self.eng[e].wait_ge(sem, val)
                self.waited[k] = val
                self.n_inst += 1

    def maybe_barrier(self):
        return

    def finish(self, e, keys):
        for k in keys:
            self._wait(e, self.lastw.get(k))


_UQ = [0]


def uq(name):
    _UQ[0] += 1
    return "%s_u%d" % (name, _UQ[0])


class Pool:
    def __init__(self, nc, stack, name, shape, dtype, n, psum=False):
        self.bufs = []
        name = uq(name)
        for i in range(n):
            nm = "%s_%d" % (name, i)
            if psum:
                t = stack.enter_context(nc.psum_tensor(nm, shape, dtype))
            else:
                t = stack.enter_context(nc.sbuf_tensor(nm, shape, dtype))
            self.bufs.append((t, nm))
        self.i = 0

    def get(self):
        b = self.bufs[self.i % len(self.bufs)]
        self.i += 1
        return b


def host_consts():
    c = {}
    c["ident"] = np.eye(128, dtype=np.float32)
    c["ones"] = np.ones((128, 128), dtype=np.float32)
    rows = SEQ // 64
    row = np.repeat(np.arange(rows, dtype=np.float32), 64)
    col = np.tile(np.arange(64, dtype=np.float32), rows)
    inv_freq = (np.float32(10000.0) ** (-np.arange(0, 64, 2, dtype=np.float32) / np.float32(64))).astype(np.float32)
    ang_r = row[:, None] * inv_freq[None, :]
    ang_c = col[:, None] * inv_freq[None, :]
    ang = np.concatenate([ang_r, ang_r, ang_c, ang_c], axis=-1).astype(np.float32)
    c["cosT"] = np.ascontiguousarray(np.cos(ang).astype(np.float32).T)
    c["sinT"] = np.ascontiguousarray(np.sin(ang).astype(np.float32).T)
    rot = np.zeros((128, 128), np.float32)
    for d in range(128):
        if d % 64 < 32:
            rot[d + 32, d] = -1.0
        else:
            rot[d - 32, d] = 1.0
    c["rotm"] = rot
    wm = np.zeros((128, 6, 512), np.float32)
    kk = np.arange(128)[:, None]
    qq = np.arange(128)[None, :]
    for r in range(-1, 5):
        for j in range(4):
            dlt = r - j
            if dlt == 0:
                m = np.ones((128, 128), np.float32)
            elif dlt == -1:
                m = (qq <= kk).astype(np.float32)
            elif dlt == 1:
                m = (kk <= qq).astype(np.float32)
            else:
                m = np.zeros((128, 128), np.float32)
            wm[:, r + 1, j * 128:(j + 1) * 128] = m
    c["wmask"] = wm
    ii = np.arange(128)[:, None]
    jj = np.arange(128)[None, :]
    c["tri_f"] = (ii <= jj).astype(np.float32)
    c["tri_b"] = (ii >= jj).astype(np.float32)
    c["negm_f"] = np.where(jj <= ii, 0.0, -30000.0).astype(np.float32)
    c["negm_b"] = np.where(jj >= ii, 0.0, -30000.0).astype(np.float32)
    c["offdiag"] = (1.0 - np.eye(128)).astype(np.float32)
    c["sut"] = (ii < jj).astype(np.float32)
    c["iota64"] = np.tile(np.arange(64, dtype=np.float32)[None, :], (128, 1))
    c["blk128"] = np.tile((float(GRAN) * np.arange(NBIG, dtype=np.float32))[None, :], (64, 1))
    c["pcol"] = np.arange(128, dtype=np.float32)[:, None].copy()
    return c


CONST_SHAPES = {"ident": [128, 128], "ones": [128, 128], "cosT": [128, SEQ], "sinT": [128, SEQ],
                "rotm": [128, 128], "wmask": [128, 6, 512], "tri_f": [128, 128], "tri_b": [128, 128],
                "negm_f": [128, 128], "negm_b": [128, 128], "offdiag": [128, 128],
                "sut": [128, 128], "iota64": [128, 64], "blk128": [64, NBIG], "pcol": [128, 1]}

W_SHAPES = {
    "w_mod": [DEPTH, D, MODW], "b_mod": [DEPTH, MODW], "w_in": [DEPTH, D, IN_W],
    "q_norm_a": [DEPTH, HD], "k_norm_a": [DEPTH, HD], "sink_b": [DEPTH, 8],
    "conv_c": [DEPTH, 5, 2048], "a_log_c": [DEPTH, 2, 8], "dt_bias_c": [DEPTH, 2, 8],
    "norm_c": [DEPTH, HD], "w_br_a": [DEPTH, 1024, D], "w_br_b": [DEPTH, 1024, D],
    "w_br_c": [DEPTH, 1024, D], "w_out": [DEPTH, D, D], "ln1_g": [DEPTH, D], "ln1_b": [DEPTH, D],
    "w_router": [DEPTH, D, NE], "router_bias": [DEPTH, NE],
    "w1r": [DEPTH * NE * 128, 8192], "w3r": [DEPTH * NE * 128, 8192], "w2r": [DEPTH * NE * 128, 8192],
    "ws1": [DEPTH, D, EFF], "ws3": [DEPTH, D, EFF], "ws2": [DEPTH, EFF, D],
    "ln2_g": [DEPTH, D], "ln2_b": [DEPTH, D],
}


class LazyIn(dict):
    def __init__(self, nc, base):
        super().__init__(base)
        self.nc = nc
        self.used = []

    def __missing__(self, k):
        shp = CONST_SHAPES[k] if k in CONST_SHAPES else W_SHAPES[k]
        ap = self.nc.dram_tensor(k, shp, F32, kind="ExternalInput").ap()
        self[k] = ap
        self.used.append(k)
        return ap


class Builder:
    def __init__(self, dbg=()):
        self.dbg = set(dbg)
        self.nc = bass.Bass("TRN2", target_bir_lowering=False)
        self.outs = []

    def dram(self, name, shape, dtype=F32):
        kind = "ExternalOutput" if name in self.dbg else "Internal"
        if name in self.dbg:
            self.outs.append(name)
        return self.nc.dram_tensor(name, shape, dtype, kind=kind).ap()

    def ln_rows(self, S, xt, key, small, n=128, gb=None, junk=None):
        nc = self.nc
        st, stk = small.get()
        sq, sqk = (junk or self.junk).get()
        S.op("dve", nc.vector.reduce_sum, st[:n, 0:1], xt[:n, :], AX.X, reads=[key], writes=[stk])
        S.op("dve", nc.vector.tensor_scalar, st[:n, 1:2], st[:n, 0:1], -1.0 / D, None, ALU.mult,
             reads=[stk], writes=[stk])
        S.op("dve", nc.vector.tensor_scalar, xt[:n, :], xt[:n, :], st[:n, 1:2], None, ALU.add,
             reads=[stk, key], writes=[key])
        S.op("act", nc.scalar.activation, sq[:n, :], xt[:n, :], AF.Square, accum_out=st[:n, 2:3],
             reads=[key], writes=[sqk, stk])
        S.op("act", nc.scalar.activation, st[:n, 3:4], st[:n, 2:3], AF.Sqrt, bias=LN_EPS, scale=1.0 / D,
             reads=[stk], writes=[stk])
        S.op("dve", nc.vector.reciprocal, st[:n, 4:5], st[:n, 3:4], reads=[stk], writes=[stk])
        S.op("dve", nc.vector.tensor_scalar, xt[:n, :], xt[:n, :], st[:n, 4:5], None, ALU.mult,
             reads=[stk, key], writes=[key])
        if gb is not None:
            g, b, gk, bk = gb
            S.op("dve", nc.vector.tensor_tensor, xt[:n, :], xt[:n, :], g[:n, :], ALU.mult,
                 reads=[key, gk], writes=[key])
            S.op("dve", nc.vector.tensor_tensor, xt[:n, :], xt[:n, :], b[:n, :], ALU.add,
                 reads=[key, bk], writes=[key])

    def build(self, stop_after="all"):
        nc = self.nc
        self.inp = {}
        self.inp["xin"] = nc.dram_tensor("xin", [NB, T, D], F32, kind="ExternalInput").ap()
        self.inp["cT"] = nc.dram_tensor("cT", [NB, 128, KC, 2], F32, kind="ExternalInput").ap()
        self.inp = LazyIn(nc, self.inp)
        self.out_all = nc.dram_tensor("out", [NB, SEQ, D], F32, kind="ExternalOutput").ap()
        self.bi = 0
        self.out = self.out_all[0]
        self.xs = self.dram("xs", [T, D])
        self.modd = self.dram("modd", [DEPTH, 2, MODW])
        self.PT = self.dram("PT", [IN_W, T])
        self.QK = self.dram("QK", [20, 128, T], BF16)
        self.OT = self.dram("OT", [3072, T], BF16)
        self.CQT = self.dram("CQT", [4, 128, T])
        self.CKT = self.dram("CKT", [4, 128, T])
        self.CKtm = self.dram("CKtm", [4, T, 128])
        self.CVtm = self.dram("CVtm", [8, T, 128])
        self.OC = [self.dram("OC0", [T, 1024]), self.dram("OC1", [T, 1024])]
        self.YP = self.dram("YP", [D, T], BF16)
        self.XS = self.dram("XSORT", [NSLOT, D], BF16)
        self.YS = self.dram("YSORT", [NSLOT, D], BF16)

        with ExitStack() as st:
            S = Sched(nc, st)
            self.S = S
            self.st = st
            self.psA = Pool(nc, st, "psA", [128, 512], F32, 6, psum=True)
            self.psB = Pool(nc, st, "psB", [128, 512], F32, 2, psum=True)
            self.small = Pool(nc, st, "small", [128, 8], F32, 4)
            self.ident = st.enter_context(nc.sbuf_tensor(uq("sb_ident"), [128, 128], F32))
            self.ones = st.enter_context(nc.sbuf_tensor(uq("sb_ones"), [128, 128], F32))
            self.onesb = st.enter_context(nc.sbuf_tensor(uq("sb_onesb"), [128, 128], BF16))
            S.dma("sp", self.ident[:], self.inp["ident"][:, :], writes=["ident"])
            S.dma("sp", self.ones[:], self.inp["ones"][:, :], writes=["ones"])
            S.dma("pool", self.onesb[:], self.inp["ones"][:, :], writes=["onesb"])
            self.modc = st.enter_context(nc.sbuf_tensor(uq("sb_modc"), [128, 96, 2], F32))

            if stop_after.startswith("only_"):
                getattr(self, "stage_" + stop_after[5:])(0, True)
                S.finish("sp", ["dram_" + n for n in self.outs] + ["dram_out"])
                return nc
            for bi in range(NB):
                self.bi = bi
                self.out = self.out_all[bi]
                self.stage_entry_ln()
                for l in range(DEPTH):
                    if stop_after == "ln":
                        break
                    self.stage_mod(l)
                    if stop_after == "mod":
                        break
                    need_ctx = l < DEPTH - 1
                    self.stage_inproj(l)
                    if stop_after == "inproj":
                        break
                    self.stage_qkprep(l)
                    if stop_after == "qkprep":
                        break
                    self.stage_attn(l, "A", need_ctx)
                    self.stage_attn(l, "B", need_ctx)
                    if stop_after == "attn":
                        break
                    self.stage_delta_prep(l)
                    if stop_after == "dprep":
                        break
                    self.stage_delta_scan(l, need_ctx)
                    if stop_after == "dscan":
                        break
                    self.stage_delta_out(l, need_ctx)
                    if stop_after == "delta":
                        break
                    self.stage_merge(l, need_ctx)
                    if stop_after == "merge":
                        break
                    self.stage_wout_ln(l, need_ctx)
                    if stop_after == "ln1":
                        break
                    self.stage_moe(l, need_ctx)
                    if stop_after == "moe%d" % l:
                        break
            S.finish("sp", ["dram_" + n for n in self.outs] + ["dram_out"])
        return nc

    def stage_entry_ln(self):
        nc, S = self.nc, self.S
        with ExitStack() as st:
            xp = Pool(nc, st, "elx", [128, D], F32, 3)
            self.junk = Pool(nc, st, "junk", [128, D], F32, 1)
            for i in range(NT):
                xt, k = xp.get()
                S.dma("sp", xt[:], self.inp["xin"][self.bi, i * 128:(i + 1) * 128, :], writes=[k])
                self.ln_rows(S, xt, k, self.small)
                S.dma("act", self.xs[i * 128:(i + 1) * 128, :], xt[:], reads=[k], writes=["dram_xs"])
            S.barrier()

    def stage_mod(self, l):
        nc, S = self.nc, self.S
        with ExitStack() as st:
            cT = st.enter_context(nc.sbuf_tensor(uq("cTs"), [128, KC, 2], F32))
            sg = st.enter_context(nc.sbuf_tensor(uq("cTg"), [128, KC, 2], F32))
            S.dma("sp", cT[:], self.inp["cT"][self.bi], writes=["cTs"])
            S.op("act", nc.scalar.activation, sg[:], cT[:], AF.Sigmoid, reads=["cTs"], writes=["cTg"])
            S.op("dve", nc.vector.tensor_tensor, cT[:], cT[:], sg[:], ALU.mult, reads=["cTs", "cTg"], writes=["cTs"])
            wp = Pool(nc, st, "wmod", [128, KC, 512], F32, 2)
            bp = Pool(nc, st, "bmod", [2, 512], F32, 2)
            op_ = Pool(nc, st, "omod", [2, 512], F32, 2)
            wsrc = self.inp["w_mod"]
            for nb in range(MODW // 512):
                wt, wk = wp.get()
                S.dma("sp" if nb % 2 == 0 else "act", wt[:],
                      wsrc[l, :, nb * 512:(nb + 1) * 512].rearrange("(kc p) n -> p kc n", p=128), writes=[wk])
                bt, bk = bp.get()
                for m in range(2):
                    S.dma("sp", bt[m:m + 1, :], self.inp["b_mod"][l:l + 1, nb * 512:(nb + 1) * 512], writes=[bk])
                ps, pk = self.psA.get()
                for kc in range(KC):
                    S.op("pe", nc.tensor.matmul, ps[0:2, :], cT[:, kc, :], wt[:, kc, :],
                         start=(kc == 0), stop=(kc == KC - 1), reads=["cTs", wk], writes=[pk])
                ot, ok = op_.get()
                S.op("dve", nc.vector.tensor_tensor, ot[:], ps[0:2, :], bt[:], ALU.add, reads=[pk, bk], writes=[ok])
                S.dma("sp", self.modd[l, :, nb * 512:(nb + 1) * 512], ot[:], reads=[ok], writes=["dram_modd"])
            for m in range(2):
                S.dma("sp", self.modc[:, :, m], self.modd[l, m, :].rearrange("(j p) -> p j", p=128),
                      reads=["dram_modd"], writes=["modc"], allow_slow_non_contiguous=True)
            for j0 in (16, 64):
                S.op("dve", nc.vector.tensor_scalar, self.modc[:, j0:j0 + 16, :], self.modc[:, j0:j0 + 16, :],
                     1.0, None, ALU.add, reads=["modc"], writes=["modc"])
            S.barrier()

    def make_hT(self, hT, hk, t0, ntok, sh_j, sc_j, src, src_key):
        nc, S = self.nc, self.S
        m = 1 if t0 < NCTX else 0
        for tb in range(0, ntok, 512):
            nb = min(512, ntok - tb)
            xts = []
            for i in range(nb // 128):
                xt, k = self.xload.get()
                r0 = t0 + tb + i * 128
                S.dma("sp", xt[:], src[r0:r0 + 128, :], reads=[src_key], writes=[k])
                xts.append((xt, k))
            for kc in range(KC):
                ps, pk = self.psB.get()
                for i, (xt, k) in enumerate(xts):
                    S.op("pe", nc.tensor.transpose, ps[:, i * 128:(i + 1) * 128], xt[:, kc * 128:(kc + 1) * 128],
                         self.ident[:], reads=[k, "ident"], writes=[pk])
                S.op("act", nc.scalar.activation, hT[:, kc, tb:tb + nb], ps[:, :nb], AF.Identity,
                     bias=self.modc[:, sh_j + kc, m:m + 1], scale=self.modc[:, sc_j + kc, m:m + 1],
                     reads=[pk, "modc"], writes=[hk])

    def stage_inproj(self, l):
        nc, S = self.nc, self.S
        with ExitStack() as st:
            hp = Pool(nc, st, "hT", [128, KC, 1024], BF16, 1)
            self.xload = Pool(nc, st, "xload", [128, D], F32, 4)
            wp = Pool(nc, st, "win", [128, KC, 512], BF16, 2)
            ep = Pool(nc, st, "pev", [128, 512], F32, 4)
            groups = [(0, NCTX)] + [(NCTX + g * 1024, 1024) for g in range(4)]
            ncb = (IN_W + 511) // 512
            ei = 0
            for (t0, ntok) in groups:
                hT, hk = hp.get()
                self.make_hT(hT, hk, t0, ntok, 0, 16, self.xs, "dram_xs")
                for cb in range(ncb):
                    S.maybe_barrier()
                    c0 = cb * 512
                    cw = min(512, IN_W - c0)
                    wt, wk = wp.get()
                    S.dma("pool", wt[:, :, :cw],
                          self.inp["w_in"][l, :, c0:c0 + cw].rearrange("(kc p) n -> p kc n", p=128), writes=[wk])
                    for cc in range(0, cw, 128):
                        cn = min(128, cw - cc)
                        for tb in range(0, ntok, 512):
                            nb = min(512, ntok - tb)
                            ps, pk = self.psA.get()
                            for kc in range(KC):
                                S.op("pe", nc.tensor.matmul, ps[:cn, :nb], wt[:, kc, cc:cc + cn], hT[:, kc, tb:tb + nb],
                                     start=(kc == 0), stop=(kc == KC - 1), reads=[wk, hk], writes=[pk])
                            et, ek = ep.get()
                            eng = "act" if ei % 2 == 0 else "dve"
                            if eng == "act":
                                S.op("act", nc.scalar.copy, et[:cn, :nb], ps[:cn, :nb], reads=[pk], writes=[ek])
                            else:
                                S.op("dve", nc.vector.tensor_copy, et[:cn, :nb], ps[:cn, :nb], reads=[pk], writes=[ek])
                            ei += 1
                            S.dma("sp", self.PT[c0 + cc:c0 + cc + cn, t0 + tb:t0 + tb + nb], et[:cn, :nb],
                                  reads=[ek], writes=["dram_PT"])
            S.barrier()


    def stage_qkprep(self, l):
        nc, S = self.nc, self.S
        with ExitStack() as st:
            cosT = st.enter_context(nc.sbuf_tensor(uq("sb_cosT"), [128, SEQ], F32))
            sinT = st.enter_context(nc.sbuf_tensor(uq("sb_sinT"), [128, SEQ], F32))
            rotm = st.enter_context(nc.sbuf_tensor(uq("sb_rotm"), [128, 128], F32))
            gq = st.enter_context(nc.sbuf_tensor(uq("sb_gq"), [128, 2], F32))
            S.dma("sp", cosT[:], self.inp["cosT"][:, :], writes=["cosT"])
            S.dma("act", sinT[:], self.inp["sinT"][:, :], writes=["sinT"])
            S.dma("sp", rotm[:], self.inp["rotm"][:, :], writes=["rotm"])
            S.dma("sp", gq[:, 0:1], self.inp["q_norm_a"][l, :].rearrange("(p o) -> p o", o=1), writes=["gq"])
            S.dma("sp", gq[:, 1:2], self.inp["k_norm_a"][l, :].rearrange("(p o) -> p o", o=1), writes=["gq"])
            S.op("dve", nc.vector.tensor_scalar, gq[:, 0:1], gq[:, 0:1], HD ** -0.5, None, ALU.mult,
                 reads=["gq"], writes=["gq"])
            xp = Pool(nc, st, "qx", [128, 512], F32, 3)
            sqp = Pool(nc, st, "qsq", [128, 512], F32, 2)
            rp = Pool(nc, st, "qr", [128, 512], F32, 2)
            xnp = Pool(nc, st, "qxn", [128, 512], F32, 2)
            t1p = Pool(nc, st, "qt1", [128, 512], F32, 2)
            t2p = Pool(nc, st, "qt2", [128, 512], F32, 2)
            obp = Pool(nc, st, "qob", [128, 512], BF16, 3)
            sc = HD ** -0.5
            heads = [(O_AQ + h * 128, True, 0, 1.0) for h in range(8)]
            heads += [(O_AK + g * 128, True, 1, 1.0) for g in range(2)]
            heads += [(O_BQ + h * 128, False, 0, sc) for h in range(8)]
            heads += [(O_BK + g * 128, False, 0, 1.0) for g in range(2)]
            blocks = [(0, NCTX)] + [(NCTX + i * 512, 512) for i in range(8)]
            for hi, (row0, rms, gcol, scl) in enumerate(heads):
                S.maybe_barrier()
                for (t0, nb) in blocks:
                    x, xk = xp.get()
                    S.dma("sp", x[:, :nb], self.PT[row0:row0 + 128, t0:t0 + nb], reads=["dram_PT"], writes=[xk])
                    if rms:
                        sq, sqk = sqp.get()
                        S.op("act", nc.scalar.activation, sq[:, :nb], x[:, :nb], AF.Square, reads=[xk], writes=[sqk])
                        ps, pk = self.psA.get()
                        S.op("pe", nc.tensor.matmul, ps[:, :nb], self.ones[:], sq[:, :nb], start=True, stop=True,
                             reads=["ones", sqk], writes=[pk])
                        r, rk = rp.get()
                        S.op("act", nc.scalar.activation, r[:, :nb], ps[:, :nb], AF.Sqrt, bias=RMS_EPS, scale=1.0 / HD,
                             reads=[pk], writes=[rk])
                        S.op("dve", nc.vector.reciprocal, r[:, :nb], r[:, :nb], reads=[rk], writes=[rk])
                        xn, xnk = xnp.get()
                        S.op("dve", nc.vector.scalar_tensor_tensor, xn[:, :nb], x[:, :nb], gq[:, gcol:gcol + 1], r[:, :nb],
                             ALU.mult, ALU.mult, reads=[xk, "gq", rk], writes=[xnk])
                    else:
                        xn, xnk = x, xk
                    ob, obk = obp.get()
                    if t0 >= NCTX:
                        p0 = t0 - NCTX
                        ps2, pk2 = self.psA.get()
                        S.op("pe", nc.tensor.matmul, ps2[:, :nb], rotm[:], xn[:, :nb], start=True, stop=True,
                             reads=["rotm", xnk], writes=[pk2])
                        t1, t1k = t1p.get()
                        S.op("dve", nc.vector.scalar_tensor_tensor, t1[:, :nb], xn[:, :nb], scl, cosT[:, p0:p0 + nb],
                             ALU.mult, ALU.mult, reads=[xnk, "cosT"], writes=[t1k])
                        t2, t2k = t2p.get()
                        S.op("dve", nc.vector.scalar_tensor_tensor, t2[:, :nb], ps2[:, :nb], scl, sinT[:, p0:p0 + nb],
                             ALU.mult, ALU.mult, reads=[pk2, "sinT"], writes=[t2k])
                        S.op("dve", nc.vector.tensor_tensor, ob[:, :nb], t1[:, :nb], t2[:, :nb], ALU.add,
                             reads=[t1k, t2k], writes=[obk])
                    else:
                        S.op("act", nc.scalar.mul, ob[:, :nb], xn[:, :nb], scl, reads=[xnk], writes=[obk])
                    S.dma("act", self.QK[hi, :, t0:t0 + nb], ob[:, :nb], reads=[obk], writes=["dram_QK"])
            S.barrier()

    def stage_attn(self, l, which, need_ctx):
        nc, S = self.nc, self.S
        isB = which == "B"
        base = 10 if isB else 0
        vrow = O_BV if isB else O_AV
        orow = 1024 if isB else 0
        with ExitStack() as st:
            kT = st.enter_context(nc.sbuf_tensor(uq("sb_kT"), [128, T], BF16))
            vsb = st.enter_context(nc.sbuf_tensor(uq("sb_vsb"), [128, NT, 128], BF16))
            vlp = Pool(nc, st, "vl", [128, 512], F32, 2)
            qp = Pool(nc, st, "aq", [128, 512], BF16, 2)
            pp = Pool(nc, st, "ap", [128, 512], BF16, 4)
            rdp = Pool(nc, st, "ard", [128, 512], F32, 2)
            otp = Pool(nc, st, "aot", [128, 512], BF16, 2)
            if isB:
                wmask = st.enter_context(nc.sbuf_tensor(uq("sb_wmask"), [128, 6, 512], BF16))
                esink = st.enter_context(nc.sbuf_tensor(uq("sb_esink"), [128, 8], F32))
                S.dma("pool", wmask[:], self.inp["wmask"][:, :, :], writes=["wmask"])
                S.dma("sp", esink[:], self.inp["sink_b"][l, :].partition_broadcast(128), writes=["esink"])
                S.op("act", nc.scalar.activation, esink[:], esink[:], AF.Exp, reads=["esink"], writes=["esink"])
            vblocks = [(0, NCTX)] + [(NCTX + i * 512, 512) for i in range(8)]
            for g in range(2):
                S.dma("sp", kT[:], self.QK[base + 8 + g, :, :], reads=["dram_QK"], writes=["kT"])
                for (t0, nb) in vblocks:
                    vl, vk = vlp.get()
                    S.dma("sp", vl[:, :nb], self.PT[vrow + g * 128:vrow + (g + 1) * 128, t0:t0 + nb],
                          reads=["dram_PT"], writes=[vk])
                    ps, pk = self.psA.get()
                    for j in range(nb // 128):
                        S.op("pe", nc.tensor.transpose, ps[:, j * 128:(j + 1) * 128], vl[:, j * 128:(j + 1) * 128],
                             self.ident[:], reads=[vk, "ident"], writes=[pk])
                    tt0 = t0 // 128
                    S.op("act", nc.scalar.copy, vsb[:, tt0:tt0 + nb // 128, :],
                         ps[:, :nb].rearrange("p (j d) -> p j d", d=128), reads=[pk], writes=["vsb"])
                for hh in range(4):
                    S.maybe_barrier()
                    h = g * 4 + hh
                    qblocks = ([(0, NCTX, True)] if need_ctx else []) + [(NCTX + i * 512, 512, False) for i in range(8)]
                    for (t0, nq, isctx) in qblocks:
                        q, qk = qp.get()
                        S.dma("sp", q[:, :nq], self.QK[base + h, :, t0:t0 + nq], reads=["dram_QK"], writes=[qk])
                        if isctx:
                            tiles = [(0, None), (1, None)]
                        elif not isB:
                            tiles = [(kt, None) for kt in range(NT)]
                        else:
                            ql0 = (t0 - NCTX) // 128
                            tiles = [(0, None), (1, None)]
                            for kl in range(ql0 - 1, ql0 + 5):
                                if 0 <= kl < SEQ // 128:
                                    tiles.append((2 + kl, kl - ql0 + 1))
                        po, pok = self.psB.get()
                        pd, pdk = self.psB.get()
                        for ti, (kt, mk) in enumerate(tiles):
                            ps, pk = self.psA.get()
                            S.op("pe", nc.tensor.matmul, ps[:, :nq], kT[:, kt * 128:(kt + 1) * 128], q[:, :nq],
                                 start=True, stop=True, reads=["kT", qk], writes=[pk])
                            p, ppk = pp.get()
                            S.op("act", nc.scalar.activation, p[:, :nq], ps[:, :nq], AF.Exp, reads=[pk], writes=[ppk])
                            if mk is not None and mk != 1 + 0 and False:
                                pass
                            if mk is not None:
                                S.op("pool", nc.gpsimd.tensor_tensor, p[:, :nq], p[:, :nq], wmask[:, mk, :nq], ALU.mult,
                                     reads=[ppk, "wmask"], writes=[ppk])
                            first, last = ti == 0, ti == len(tiles) - 1
                            S.op("pe", nc.tensor.matmul, po[:, :nq], vsb[:, kt, :], p[:, :nq], start=first, stop=last,
                                 reads=["vsb", ppk], writes=[pok])
                            S.op("pe", nc.tensor.matmul, pd[:, :nq], self.onesb[:], p[:, :nq], start=first, stop=last,
                                 reads=["onesb", ppk], writes=[pdk])
                        rd, rdk = rdp.get()
                        if isB:
                            S.op("dve", nc.vector.tensor_scalar, rd[:, :nq], pd[:, :nq], esink[:, h:h + 1], None, ALU.add,
                                 reads=[pdk, "esink"], writes=[rdk])
                            S.op("dve", nc.vector.reciprocal, rd[:, :nq], rd[:, :nq], reads=[rdk], writes=[rdk])
                        else:
                            S.op("dve", nc.vector.reciprocal, rd[:, :nq], pd[:, :nq], reads=[pdk], writes=[rdk])
                        ot, otk = otp.get()
                        S.op("dve", nc.vector.tensor_tensor, ot[:, :nq], po[:, :nq], rd[:, :nq], ALU.mult,
                             reads=[pok, rdk], writes=[otk])
                        S.dma("act", self.OT[orow + h * 128:orow + (h + 1) * 128, t0:t0 + nq], ot[:, :nq],
                              reads=[otk], writes=["dram_OT"])
            S.barrier()


    def stage_delta_prep(self, l):
        nc, S = self.nc, self.S
        with ExitStack() as st:
            wc = st.enter_context(nc.sbuf_tensor(uq("sb_wc"), [128, 16, 5], F32))
            for j in range(5):
                S.dma("sp", wc[:, :, j], self.inp["conv_c"][l, j, :].rearrange("(c p) -> p c", p=128), writes=["wc"],
                      allow_slow_non_contiguous=True)
            xpp = Pool(nc, st, "cxp", [128, T + 8], F32, 2)
            acp = Pool(nc, st, "cac", [128, T], F32, 2)
            sqp = Pool(nc, st, "csq", [128, 512], F32, 2)
            rp = Pool(nc, st, "cr", [128, 512], F32, 2)
            tmp = Pool(nc, st, "ctm", [128, 4, 128], F32, 3)
            for (xp_, k) in xpp.bufs:
                S.op("pool", nc.gpsimd.memset, xp_[:], 0.0, writes=[k])
            blocks = [(0, NCTX)] + [(NCTX + i * 512, 512) for i in range(8)]
            for fc in range(16):
                row0 = O_CQ + fc * 128
                xp_, xk = xpp.get()
                S.dma("sp", xp_[:, 2:2 + NCTX], self.PT[row0:row0 + 128, 0:NCTX], reads=["dram_PT"], writes=[xk])
                S.dma("act", xp_[:, 262:262 + SEQ], self.PT[row0:row0 + 128, NCTX:T], reads=["dram_PT"], writes=[xk])
                ac, ak = acp.get()
                for (o0, n, s0) in ((0, NCTX, 0), (NCTX, SEQ, 260)):
                    S.op("dve", nc.vector.tensor_scalar, ac[:, o0:o0 + n], xp_[:, s0:s0 + n], wc[:, fc, 0:1], None, ALU.mult,
                         reads=[xk, "wc"], writes=[ak])
                    for j in range(1, 5):
                        S.op("dve", nc.vector.scalar_tensor_tensor, ac[:, o0:o0 + n], xp_[:, s0 + j:s0 + j + n],
                             wc[:, fc, j:j + 1], ac[:, o0:o0 + n], ALU.mult, ALU.add, reads=[xk, "wc", ak], writes=[ak])
                S.op("act", nc.scalar.activation, ac[:, :], ac[:, :], AF.Silu, reads=[ak], writes=[ak])
                if fc < 8:
                    scl = HD ** -0.5 if fc < 4 else 1.0
                    for (t0, nb) in blocks:
                        sq, sqk = sqp.get()
                        S.op("act", nc.scalar.activation, sq[:, :nb], ac[:, t0:t0 + nb], AF.Square, reads=[ak], writes=[sqk])
                        ps, pk = self.psA.get()
                        S.op("pe", nc.tensor.matmul, ps[:, :nb], self.ones[:], sq[:, :nb], start=True, stop=True,
                             reads=["ones", sqk], writes=[pk])
                        r, rk = rp.get()
                        S.op("act", nc.scalar.activation, r[:, :nb], ps[:, :nb], AF.Sqrt, bias=RMS_EPS, scale=1.0,
                             reads=[pk], writes=[rk])
                        S.op("dve", nc.vector.reciprocal, r[:, :nb], r[:, :nb], reads=[rk], writes=[rk])
                        S.op("dve", nc.vector.scalar_tensor_tensor, ac[:, t0:t0 + nb], ac[:, t0:t0 + nb], scl, r[:, :nb],
                             ALU.mult, ALU.mult, reads=[ak, rk], writes=[ak])
                    dst = self.CQT if fc < 4 else self.CKT
                    S.dma("sp", dst[fc % 4, :, :], ac[:, :], reads=[ak], writes=["dram_CQKT"])
                if fc >= 4:
                    dst = self.CKtm[fc - 4] if fc < 8 else self.CVtm[fc - 8]
                    for (t0, nb) in blocks:
                        ps, pk = self.psA.get()
                        nj = nb // 128
                        for j in range(nj):
                            S.op("pe", nc.tensor.transpose, ps[:, j * 128:(j + 1) * 128],
                                 ac[:, t0 + j * 128:t0 + (j + 1) * 128], self.ident[:], reads=[ak, "ident"], writes=[pk])
                        tm, tk = tmp.get()
                        S.op("act", nc.scalar.copy, tm[:, :nj, :], ps[:, :nb].rearrange("p (j d) -> p j d", d=128),
                             reads=[pk], writes=[tk])
                        S.dma("sp", dst[t0:t0 + nb, :].rearrange("(j t) d -> t j d", t=128), tm[:, :nj, :],
                              reads=[tk], writes=["dram_Ctm"])
            S.barrier()

    def stage_delta_scan(self, l, need_ctx):
        nc, S = self.nc, self.S
        NCH = NT
        with ExitStack() as st:
            def sb(name, shape, dt=F32):
                return st.enter_context(nc.sbuf_tensor(uq(name), shape, dt))
            tri = [sb("sb_trif", [128, 128]), sb("sb_trib", [128, 128])]
            negm = [sb("sb_negf", [128, 128]), sb("sb_negb", [128, 128])]
            offd = sb("sb_offd", [128, 128])
            for t_, nm in ((tri[0], "tri_f"), (tri[1], "tri_b"), (negm[0], "negm_f"), (negm[1], "negm_b"), (offd, "offdiag")):
                S.dma("sp", t_[:], self.inp[nm][:, :], writes=["dconst"])
            ab = sb("sb_ab", [32, T])
            S.dma("sp", ab[:], self.PT[O_CA:O_CA + 32, :], reads=["dram_PT"], writes=["ab"])
            abtm = sb("sb_abtm", [128, NCH, 32])
            G = sb("sb_G", [128, NCH, 16])
            Bt = sb("sb_Bt", [128, NCH, 16])
            GC = sb("sb_GC", [128, NCH, 16])
            TOT = sb("sb_TOT", [128, NCH, 16])
            EG = sb("sb_EG", [128, NCH, 16])
            BG = sb("sb_BG", [128, NCH, 16])
            EKD = sb("sb_EKD", [128, NCH, 16])
            ETOT = sb("sb_ETOT", [128, NCH, 16])
            dtb = sb("sb_dtb", [128, 16])
            nal = sb("sb_nal", [128, 16])
            S.dma("sp", dtb[:], self.inp["dt_bias_c"][l].rearrange("a b -> (a b)").partition_broadcast(128), writes=["dtb"])
            S.dma("sp", nal[:], self.inp["a_log_c"][l].rearrange("a b -> (a b)").partition_broadcast(128), writes=["nal"])
            S.op("act", nc.scalar.activation, nal[:], nal[:], AF.Exp, reads=["nal"], writes=["nal"])
            S.op("dve", nc.vector.tensor_scalar, nal[:], nal[:], -1.0, None, ALU.mult, reads=["nal"], writes=["nal"])
            for c0 in range(0, NCH, 16):
                n = min(16, NCH - c0)
                ps, pk = self.psA.get()
                for c in range(n):
                    S.op("pe", nc.tensor.transpose, ps[:, c * 32:(c + 1) * 32], ab[0:32, (c0 + c) * 128:(c0 + c + 1) * 128],
                         self.ident[0:32, 0:32], reads=["ab", "ident"], writes=[pk])
                S.op("act", nc.scalar.copy, abtm[:, c0:c0 + n, :], ps[:, :n * 32].rearrange("p (c k) -> p c k", k=32),
                     reads=[pk], writes=["abtm"])
            for c in range(NCH):
                S.op("dve", nc.vector.tensor_tensor, G[:, c, :], abtm[:, c, 0:16], dtb[:], ALU.add,
                     reads=["abtm", "dtb"], writes=["G"])
            S.op("act", nc.scalar.activation, G[:], G[:], AF.Exp, reads=["G"], writes=["G"])
            S.op("act", nc.scalar.activation, G[:], G[:], AF.Ln, bias=1.0, reads=["G"], writes=["G"])
            for c in range(NCH):
                S.op("dve", nc.vector.tensor_tensor, G[:, c, :], G[:, c, :], nal[:], ALU.mult,
                     reads=["G", "nal"], writes=["G"])
            S.op("act", nc.scalar.activation, Bt[:], abtm[:, :, 16:32], AF.Sigmoid, reads=["abtm"], writes=["Bt"])
            for c0 in range(0, NCH, 32):
                n = min(32, NCH - c0)
                ps, pk = self.psA.get()
                ps2, pk2 = self.psA.get()
                for c in range(n):
                    for d in range(2):
                        S.op("pe", nc.tensor.matmul, ps[:, c * 16 + d * 8:c * 16 + d * 8 + 8], tri[d][:],
                             G[:, c0 + c, d * 8:d * 8 + 8], start=True, stop=True, reads=["dconst", "G"], writes=[pk])
                    S.op("pe", nc.tensor.matmul, ps2[:, c * 16:(c + 1) * 16], self.ones[:], G[:, c0 + c, :],
                         start=True, stop=True, reads=["ones", "G"], writes=[pk2])
                S.op("act", nc.scalar.copy, GC[:, c0:c0 + n, :], ps[:, :n * 16].rearrange("p (c k) -> p c k", k=16),
                     reads=[pk], writes=["GC"])
                S.op("act", nc.scalar.copy, TOT[:, c0:c0 + n, :], ps2[:, :n * 16].rearrange("p (c k) -> p c k", k=16),
                     reads=[pk2], writes=["TOT"])
            S.op("act", nc.scalar.activation, EG[:], GC[:], AF.Exp, reads=["GC"], writes=["EG"])
            S.op("dve", nc.vector.tensor_tensor, BG[:], Bt[:], EG[:], ALU.mult, reads=["Bt", "EG"], writes=["BG"])
            S.op("dve", nc.vector.tensor_tensor, EKD[:], TOT[:], GC[:], ALU.subtract, reads=["TOT", "GC"], writes=["EKD"])
            S.op("act", nc.scalar.activation, EKD[:], EKD[:], AF.Exp, reads=["EKD"], writes=["EKD"])
            S.op("act", nc.scalar.activation, ETOT[:], TOT[:], AF.Exp, reads=["TOT"], writes=["ETOT"])
            gk = ["GC", "Bt", "EG", "BG", "EKD", "ETOT"]
            if "GDBG" in self.dbg:
                gd = self.dram("GDBG", [6, 128, NCH * 16])
                for i_, t_ in enumerate((GC, Bt, EG, BG, EKD, ETOT)):
                    S.dma("sp", gd[i_], t_[:].rearrange("p c k -> p (c k)"), reads=gk, writes=["dram_GDBG"])
            if os.environ.get("MK_SCAN", "full") == "gates":
                S.barrier()
                return
            nchunks_dbg = int(os.environ.get("MK_SCAN_N", "1000"))
            bi = [0]
            banks = self.psA.bufs + self.psB.bufs

            def BANK():
                r = banks[bi[0] % len(banks)]
                bi[0] += 1
                return r
            roles = ["dg", "t", "Di", "Dm", "QKd", "vb", "kbg", "kdec", "u", "wT", "vn", "o1s"]
            P = {r: Pool(nc, st, "d_" + r, [128, 4, 128], F32, 2) for r in roles}
            for r in ("X", "XT", "TT"):
                P[r] = Pool(nc, st, "d_" + r, [128, 4, 128], F32, 4)
            ldq = Pool(nc, st, "d_qT", [128, 4, 128], F32, 2)
            ldk = Pool(nc, st, "d_kT", [128, 4, 128], F32, 2)
            ldkt = Pool(nc, st, "d_ktm", [128, 4, 128], F32, 2)
            ldv = Pool(nc, st, "d_vtm", [128, 8, 128], F32, 2)
            kkp = Pool(nc, st, "d_KK", [128, 8, 128], F32, 2)
            qkp = Pool(nc, st, "d_QKT", [128, 8, 128], F32, 2)
            obp = Pool(nc, st, "d_ob", [128, 8, 128], F32, 2)
            Sst = sb("sb_state", [128, 8, 128])
            ident4 = sb("sb_ident4", [128, 4, 128])
            offd4 = sb("sb_offd4", [128, 4, 128])
            negm4 = [sb("sb_negm4f", [128, 4, 128]), sb("sb_negm4b", [128, 4, 128])]
            for i in range(4):
                S.op("pool", nc.gpsimd.tensor_copy, ident4[:, i, :], self.ident[:], reads=["ident"], writes=["dconst4"])
                S.op("pool", nc.gpsimd.tensor_copy, offd4[:, i, :], offd[:], reads=["dconst"], writes=["dconst4"])
                for d in range(2):
                    S.op("pool", nc.gpsimd.tensor_copy, negm4[d][:, i, :], negm[d][:], reads=["dconst"], writes=["dconst4"])
            ident, ones = self.ident, self.ones

            def flat(t):
                return t[:].rearrange("p h d -> p (h d)")

            for d in range(2):
                order = [0, 1] + list(range(2, NCH)) if d == 0 else [1, 0] + list(range(NCH - 1, 1, -1))
                S.op("pool", nc.gpsimd.memset, Sst[:], 0.0, writes=["S0", "S1"])
                for c in order[:nchunks_dbg]:
                    t0 = c * 128
                    qT, qTk = ldq.get()
                    kT, kTk = ldk.get()
                    ktm, ktmk = ldkt.get()
                    vtm, vtmk = ldv.get()
                    S.dma("sp", qT[:], self.CQT[:, :, t0:t0 + 128].rearrange("h d t -> d h t"), reads=["dram_CQKT"], writes=[qTk])
                    S.dma("sp", kT[:], self.CKT[:, :, t0:t0 + 128].rearrange("h d t -> d h t"), reads=["dram_CQKT"], writes=[kTk])
                    S.dma("act", ktm[:], self.CKtm[:, t0:t0 + 128, :].rearrange("h t d -> t h d"), reads=["dram_Ctm"], writes=[ktmk])
                    S.dma("act", vtm[:], self.CVtm[:, t0:t0 + 128, :].rearrange("h t d -> t h d"), reads=["dram_Ctm"], writes=[vtmk])
                    KK, KKk = kkp.get()
                    QKT, QKTk = qkp.get()
                    bk1, bk1k = BANK()
                    for hq in range(4):
                        S.op("pe", nc.tensor.matmul, bk1[:, hq * 128:(hq + 1) * 128], kT[:, hq, :], kT[:, hq, :],
                             start=True, stop=True, reads=[kTk], writes=[bk1k])
                    bk2, bk2k = BANK()
                    for hq in range(4):
                        S.op("pe", nc.tensor.matmul, bk2[:, hq * 128:(hq + 1) * 128], kT[:, hq, :], qT[:, hq, :],
                             start=True, stop=True, reads=[kTk, qTk], writes=[bk2k])
                    for rep in range(2):
                        S.op("act", nc.scalar.copy, KK[:].rearrange("p (h r) d -> p h r d", r=2)[:, :, rep, :],
                             bk1[:, :].rearrange("p (h d) -> p h d", d=128), reads=[bk1k], writes=[KKk])
                        S.op("dve", nc.vector.tensor_copy, QKT[:].rearrange("p (h r) d -> p h r d", r=2)[:, :, rep, :],
                             bk2[:, :].rearrange("p (h d) -> p h d", d=128), reads=[bk2k], writes=[QKTk])
                    ob, obk = obp.get()
                    PH = float(os.environ.get("MK_SCAN_PH", "99"))
                    for g in range(2):
                        if PH < 1:
                            break
                        hvs = [g * 4 + i for i in range(4)]
                        cols = [d * 8 + hv for hv in hvs]
                        sk = "S%d" % g
                        dg, dgk = P["dg"].get()
                        for i in range(4):
                            S.op("act", nc.scalar.activation, dg[:, i, :], ident[:], AF.Identity, scale=GC[:, c, cols[i]:cols[i] + 1],
                                 reads=["ident"] + gk, writes=[dgk])
                        b1, b1k = BANK()
                        for i in range(4):
                            S.op("pe", nc.tensor.matmul, b1[:, i * 128:(i + 1) * 128], ones[:], dg[:, i, :], start=True, stop=True,
                                 reads=["ones", dgk], writes=[b1k])
                        tt_, ttk = P["t"].get()
                        S.op("dve", nc.vector.scalar_tensor_tensor, flat(tt_), b1[:, :], -1.0, flat(negm4[d]), ALU.mult, ALU.add,
                             reads=[b1k, "dconst4"], writes=[ttk])
                        Di, Dik = P["Di"].get()
                        for i in range(4):
                            S.op("act", nc.scalar.activation, Di[:, i, :], tt_[:, i, :], AF.Exp, bias=GC[:, c, cols[i]:cols[i] + 1],
                                 reads=[ttk] + gk, writes=[Dik])
                        Dm, Dmk = P["Dm"].get()
                        for i in range(4):
                            S.op("dve", nc.vector.scalar_tensor_tensor, Dm[:, i, :], Di[:, i, :], Bt[:, c, cols[i]:cols[i] + 1], offd[:],
                                 ALU.mult, ALU.mult, reads=[Dik, "dconst"] + gk, writes=[Dmk])
                        X, Xk = P["X"].get()
                        S.op("dve", nc.vector.scalar_tensor_tensor, flat(X), KK[:, g * 4:g * 4 + 4, :].rearrange("p h d -> p (h d)"), -1.0,
                             flat(Dm), ALU.mult, ALU.mult, reads=[KKk, Dmk], writes=[Xk])
                        if PH < 1.5:
                            continue
                        b2, b2k = BANK()
                        for i in range(4):
                            S.op("pe", nc.tensor.transpose, b2[:, i * 128:(i + 1) * 128], X[:, i, :], ident[:], reads=[Xk, "ident"], writes=[b2k])
                        XT, XTk = P["XT"].get()
                        S.op("act", nc.scalar.copy, flat(XT), b2[:, :], reads=[b2k], writes=[XTk])
                        if PH < 2.25:
                            continue
                        TT, TTk = P["TT"].get()
                        S.op("dve", nc.vector.tensor_tensor, flat(TT), flat(XT), flat(ident4), ALU.add, reads=[XTk, "dconst4"], writes=[TTk])
                        if PH < 2.5:
                            continue
                        b3, b3k = BANK()
                        for i in range(4):
                            S.op("pe", nc.tensor.transpose, b3[:, i * 128:(i + 1) * 128], Di[:, i, :], ident[:], reads=[Dik, "ident"], writes=[b3k])
                        if PH < 2.75:
                            continue
                        QKd, QKdk = P["QKd"].get()
                        S.op("dve", nc.vector.tensor_tensor, flat(QKd), b3[:, :], QKT[:, g * 4:g * 4 + 4, :].rearrange("p h d -> p (h d)"),
                             ALU.mult, reads=[b3k, QKTk], writes=[QKdk])
                        if PH < 3:
                            continue
                        for lev in range(1, 7):
                            b4, b4k = BANK()
                            for i in range(4):
                                S.op("pe", nc.tensor.matmul, b4[:, i * 128:(i + 1) * 128], XT[:, i, :], X[:, i, :], start=True, stop=True,
                                     reads=[XTk, Xk], writes=[b4k])
                            Xn, Xnk = P["X"].get()
                            S.op("act", nc.scalar.copy, flat(Xn), b4[:, :], reads=[b4k], writes=[Xnk])
                            if lev < 6:
                                b5, b5k = BANK()
                                for i in range(4):
                                    S.op("pe", nc.tensor.matmul, b5[:, i * 128:(i + 1) * 128], X[:, i, :], XT[:, i, :], start=True, stop=True,
                                         reads=[XTk, Xk], writes=[b5k])
                                XTn, XTnk = P["XT"].get()
                                S.op("act", nc.scalar.copy, flat(XTn), b5[:, :], reads=[b5k], writes=[XTnk])
                            b6, b6k = BANK()
                            for i in range(4):
                                S.op("pe", nc.tensor.matmul, b6[:, i * 128:(i + 1) * 128], Xn[:, i, :], TT[:, i, :], start=True, stop=True,
                                     reads=[Xnk, TTk], writes=[b6k])
                            TTn, TTnk = P["TT"].get()
                            S.op("dve", nc.vector.tensor_tensor, flat(TTn), b6[:, :], flat(TT), ALU.add, reads=[b6k, TTk], writes=[TTnk])
                            X, Xk = Xn, Xnk
                            if lev < 6:
                                XT, XTk = XTn, XTnk
                            TT, TTk = TTn, TTnk
                        if PH < 4:
                            continue
                        vb, vbk = P["vb"].get()
                        kbg, kbgk = P["kbg"].get()
                        kdec, kdeck = P["kdec"].get()
                        for i in range(4):
                            hv = hvs[i]
                            hq = hv // 2
                            S.op("pool", nc.gpsimd.tensor_scalar, vb[:, i, :], vtm[:, hv, :], Bt[:, c, cols[i]:cols[i] + 1], None, ALU.mult,
                                 reads=[vtmk] + gk, writes=[vbk])
                            S.op("pool", nc.gpsimd.tensor_scalar, kbg[:, i, :], ktm[:, hq, :], BG[:, c, cols[i]:cols[i] + 1], None, ALU.mult,
                                 reads=[ktmk] + gk, writes=[kbgk])
                            S.op("pool", nc.gpsimd.tensor_scalar, kdec[:, i, :], ktm[:, hq, :], EKD[:, c, cols[i]:cols[i] + 1], None, ALU.mult,
                                 reads=[ktmk] + gk, writes=[kdeck])
                        if PH < 5:
                            continue
                        b7, b7k = BANK()
                        for i in range(4):
                            S.op("pe", nc.tensor.matmul, b7[:, i * 128:(i + 1) * 128], TT[:, i, :], vb[:, i, :], start=True, stop=True,
                                 reads=[TTk, vbk], writes=[b7k])
                        u, uk = P["u"].get()
                        S.op("act", nc.scalar.copy, flat(u), b7[:, :], reads=[b7k], writes=[uk])
                        b8, b8k = BANK()
                        for i in range(4):
                            S.op("pe", nc.tensor.matmul, b8[:, i * 128:(i + 1) * 128], kbg[:, i, :], TT[:, i, :], start=True, stop=True,
                                 reads=[TTk, kbgk], writes=[b8k])
                        wT, wTk = P["wT"].get()
                        S.op("dve", nc.vector.tensor_copy, flat(wT), b8[:, :], reads=[b8k], writes=[wTk])
                        if PH < 6:
                            continue
                        b9, b9k = BANK()
                        for i in range(4):
                            S.op("pe", nc.tensor.matmul, b9[:, i * 128:(i + 1) * 128], wT[:, i, :], Sst[:, hvs[i], :], start=True, stop=True,
                                 reads=[wTk, sk], writes=[b9k])
                        vn, vnk = P["vn"].get()
                        S.op("dve", nc.vector.tensor_tensor, flat(vn), flat(u), b9[:, :], ALU.subtract, reads=[uk, b9k], writes=[vnk])
                        b10, b10k = BANK()
                        for i in range(4):
                            S.op("pe", nc.tensor.matmul, b10[:, i * 128:(i + 1) * 128], qT[:, hvs[i] // 2, :], Sst[:, hvs[i], :], start=True, stop=True,
                                 reads=[qTk, sk], writes=[b10k])
                        o1s, o1sk = P["o1s"].get()
                        for i in range(4):
                            S.op("act", nc.scalar.activation, o1s[:, i, :], b10[:, i * 128:(i + 1) * 128], AF.Identity,
                                 scale=EG[:, c, cols[i]:cols[i] + 1], reads=[b10k] + gk, writes=[o1sk])
                        b11, b11k = BANK()
                        for i in range(4):
                            S.op("pe", nc.tensor.matmul, b11[:, i * 128:(i + 1) * 128], QKd[:, i, :], vn[:, i, :], start=True, stop=True,
                                 reads=[QKdk, vnk], writes=[b11k])
                        S.op("dve", nc.vector.tensor_tensor, ob[:, g * 4:g * 4 + 4, :].rearrange("p h d -> p (h d)"), b11[:, :], flat(o1s), ALU.add,
                             reads=[b11k, o1sk], writes=[obk])
                        b12, b12k = BANK()
                        for i in range(4):
                            S.op("pe", nc.tensor.matmul, b12[:, i * 128:(i + 1) * 128], kdec[:, i, :], vn[:, i, :], start=True, stop=True,
                                 reads=[kdeck, vnk], writes=[b12k])
                        for i in range(4):
                            S.op("dve", nc.vector.scalar_tensor_tensor, Sst[:, hvs[i], :], Sst[:, hvs[i], :], ETOT[:, c, cols[i]:cols[i] + 1],
                                 b12[:, i * 128:(i + 1) * 128], ALU.mult, ALU.add, reads=[sk, b12k] + gk, writes=[sk])
                    if (c >= 2 or need_ctx) and PH >= 6:
                        S.dma("sp", self.OC[d][t0:t0 + 128, :], ob[:].rearrange("p h d -> p (h d)"), reads=[obk], writes=["dram_OC"])
            S.barrier()

    def stage_delta_out(self, l, need_ctx):
        nc, S = self.nc, self.S
        with ExitStack() as st:
            ncol = st.enter_context(nc.sbuf_tensor(uq("sb_ncol"), [128, 1], F32))
            S.dma("sp", ncol[:], self.inp["norm_c"][l, :].rearrange("(p o) -> p o", o=1), writes=["ncol"])
            o0p = Pool(nc, st, "go0", [128, 4, 128], F32, 2)
            o1p = Pool(nc, st, "go1", [128, 4, 128], F32, 2)
            xp = Pool(nc, st, "gx", [128, 512], F32, 2)
            sqp = Pool(nc, st, "gsq", [128, 512], F32, 2)
            rp = Pool(nc, st, "gr", [128, 512], F32, 2)
            zp = Pool(nc, st, "gz", [128, 512], F32, 2)
            obp = Pool(nc, st, "gob", [128, 512], BF16, 2)
            blocks = ([(0, NCTX)] if need_ctx else []) + [(NCTX + i * 512, 512) for i in range(8)]
            for hv in range(8):
                for (t0, nb) in blocks:
                    nj = nb // 128
                    o0, o0k = o0p.get()
                    o1, o1k = o1p.get()
                    S.dma("sp", o0[:, :nj, :], self.OC[0][t0:t0 + nb, hv * 128:(hv + 1) * 128].rearrange("(j t) d -> t j d", t=128),
                          reads=["dram_OC"], writes=[o0k])
                    S.dma("act", o1[:, :nj, :], self.OC[1][t0:t0 + nb, hv * 128:(hv + 1) * 128].rearrange("(j t) d -> t j d", t=128),
                          reads=["dram_OC"], writes=[o1k])
                    S.op("pool", nc.gpsimd.tensor_tensor, o0[:, :nj, :], o0[:, :nj, :], o1[:, :nj, :], ALU.add,
                         reads=[o0k, o1k], writes=[o0k])
                    ps, pk = self.psA.get()
                    for j in range(nj):
                        S.op("pe", nc.tensor.transpose, ps[:, j * 128:(j + 1) * 128], o0[:, j, :], self.ident[:],
                             reads=[o0k, "ident"], writes=[pk])
                    x, xk = xp.get()
                    S.op("dve", nc.vector.tensor_copy, x[:, :nb], ps[:, :nb], reads=[pk], writes=[xk])
                    sq, sqk = sqp.get()
                    S.op("act", nc.scalar.activation, sq[:, :nb], x[:, :nb], AF.Square, reads=[xk], writes=[sqk])
                    ps2, pk2 = self.psA.get()
                    S.op("pe", nc.tensor.matmul, ps2[:, :nb], self.ones[:], sq[:, :nb], start=True, stop=True,
                         reads=["ones", sqk], writes=[pk2])
                    r, rk = rp.get()
                    S.op("act", nc.scalar.activation, r[:, :nb], ps2[:, :nb], AF.Sqrt, bias=RMS_EPS, scale=1.0 / HD,
                         reads=[pk2], writes=[rk])
                    S.op("dve", nc.vector.reciprocal, r[:, :nb], r[:, :nb], reads=[rk], writes=[rk])
                    z, zk = zp.get()
                    S.dma("sp", z[:, :nb], self.PT[O_CZ + hv * 128:O_CZ + (hv + 1) * 128, t0:t0 + nb], reads=["dram_PT"], writes=[zk])
                    S.op("act", nc.scalar.activation, z[:, :nb], z[:, :nb], AF.Silu, reads=[zk], writes=[zk])
                    S.op("dve", nc.vector.scalar_tensor_tensor, x[:, :nb], x[:, :nb], ncol[:, 0:1], r[:, :nb], ALU.mult, ALU.mult,
                         reads=[xk, "ncol", rk], writes=[xk])
                    ob, obk = obp.get()
                    S.op("dve", nc.vector.tensor_tensor, ob[:, :nb], x[:, :nb], z[:, :nb], ALU.mult, reads=[xk, zk], writes=[obk])
                    S.dma("act", self.OT[2048 + hv * 128:2048 + (hv + 1) * 128, t0:t0 + nb], ob[:, :nb], reads=[obk], writes=["dram_OT"])
            S.barrier()


    def stage_merge(self, l, need_ctx):
        nc, S = self.nc, self.S
        with ExitStack() as st:
            wbr = Pool(nc, st, "wbr", [128, 8, D], BF16, 2)
            otp = Pool(nc, st, "mot", [128, 8, 512], BF16, 2)
            gp = Pool(nc, st, "mg", [128, 512], F32, 4)
            tp = Pool(nc, st, "mt", [128, 512], F32, 3)
            accp = Pool(nc, st, "macc", [128, KC, 512], F32, 1)
            ybp = Pool(nc, st, "myb", [128, KC, 512], BF16, 2)
            blocks = ([(0, NCTX)] if need_ctx else []) + [(NCTX + i * 512, 512) for i in range(8)]
            wnames = ["w_br_a", "w_br_b", "w_br_c"]
            for (t0, nb) in blocks:
                acc, acck = accp.get()
                for br in range(3):
                    wt, wk = wbr.get()
                    S.dma("pool", wt[:], self.inp[wnames[br]][l].rearrange("(kc p) n -> p kc n", p=128), writes=[wk])
                    ot, otk = otp.get()
                    S.dma("sp", ot[:, :, :nb], self.OT[br * 1024:(br + 1) * 1024, t0:t0 + nb].rearrange("(kc p) t -> p kc t", p=128),
                          reads=["dram_OT"], writes=[otk])
                    for oc in range(KC):
                        ps, pk = self.psA.get()
                        for kc in range(8):
                            S.op("pe", nc.tensor.matmul, ps[:, :nb], wt[:, kc, oc * 128:(oc + 1) * 128], ot[:, kc, :nb],
                                 start=(kc == 0), stop=(kc == 7), reads=[wk, otk], writes=[pk])
                        g, gk_ = gp.get()
                        r0 = O_G + br * D + oc * 128
                        S.dma("act" if oc % 2 else "sp", g[:, :nb], self.PT[r0:r0 + 128, t0:t0 + nb], reads=["dram_PT"], writes=[gk_])
                        S.op("act", nc.scalar.activation, g[:, :nb], g[:, :nb], AF.Sigmoid, reads=[gk_], writes=[gk_])
                        if br == 0:
                            S.op("dve", nc.vector.tensor_tensor, acc[:, oc, :nb], ps[:, :nb], g[:, :nb], ALU.mult,
                                 reads=[pk, gk_], writes=[acck])
                        else:
                            t_, tk = tp.get()
                            S.op("dve", nc.vector.tensor_tensor, t_[:, :nb], ps[:, :nb], g[:, :nb], ALU.mult,
                                 reads=[pk, gk_], writes=[tk])
                            S.op("pool", nc.gpsimd.tensor_tensor, acc[:, oc, :nb], acc[:, oc, :nb], t_[:, :nb], ALU.add,
                                 reads=[tk, acck], writes=[acck])
                yb, ybk = ybp.get()
                S.op("act", nc.scalar.copy, yb[:, :, :nb], acc[:, :, :nb], reads=[acck], writes=[ybk])
                S.dma("sp", self.YP[:, t0:t0 + nb].rearrange("(kc p) t -> p kc t", p=128), yb[:, :, :nb],
                      reads=[ybk], writes=["dram_YP"])
            S.barrier()

    def load_bc(self, st, name, src_row):
        nc, S = self.nc, self.S
        t = st.enter_context(nc.sbuf_tensor(uq("sb_bc_" + name), [128, D], F32))
        k = "bc_" + name
        S.dma("sp", t[:], src_row.partition_broadcast(128), reads=["dram_modd"], writes=[k])
        return t, k

    def stage_wout_ln(self, l, need_ctx):
        nc, S = self.nc, self.S
        with ExitStack() as st:
            wo = st.enter_context(nc.sbuf_tensor(uq("sb_wo"), [128, KC, D], BF16))
            for q in range(4):
                S.dma("pool", wo[:, :, q * 512:(q + 1) * 512],
                      self.inp["w_out"][l, :, q * 512:(q + 1) * 512].rearrange("(kc p) n -> p kc n", p=128), writes=["wo"])
            gt = [self.load_bc(st, "gt1l", self.modd[l, 0, 2 * D:3 * D]), self.load_bc(st, "gt1c", self.modd[l, 1, 2 * D:3 * D])]
            lng, lngk = self.load_bc(st, "ln1g", self.inp["ln1_g"][l, :])
            lnb, lnbk = self.load_bc(st, "ln1b", self.inp["ln1_b"][l, :])
            ybp = Pool(nc, st, "wyb", [128, KC, 512], BF16, 2)
            xp = Pool(nc, st, "wx", [128, D], F32, 2)
            zp = Pool(nc, st, "wz", [128, D], F32, 2)
            junk = Pool(nc, st, "wjunk", [128, D], F32, 1)
            blocks = ([(0, NCTX)] if need_ctx else []) + [(NCTX + i * 512, 512) for i in range(8)]
            for (t0, nb) in blocks:
                yb, ybk = ybp.get()
                S.dma("sp", yb[:, :, :nb], self.YP[:, t0:t0 + nb].rearrange("(kc p) t -> p kc t", p=128),
                      reads=["dram_YP"], writes=[ybk])
                gtt, gtk = gt[1] if t0 < NCTX else gt[0]
                for j in range(nb // 128):
                    tok0 = t0 + j * 128
                    xt, xk = xp.get()
                    S.dma("act", xt[:], self.xs[tok0:tok0 + 128, :], reads=["dram_xs"], writes=[xk])
                    zt, zk = zp.get()
                    for q in range(4):
                        ps, pk = self.psA.get()
                        for kc in range(KC):
                            S.op("pe", nc.tensor.matmul, ps[:, :], yb[:, kc, j * 128:(j + 1) * 128], wo[:, kc, q * 512:(q + 1) * 512],
                                 start=(kc == 0), stop=(kc == KC - 1), reads=[ybk, "wo"], writes=[pk])
                        S.op("dve", nc.vector.tensor_tensor, zt[:, q * 512:(q + 1) * 512], ps[:, :], gtt[:, q * 512:(q + 1) * 512],
                             ALU.mult, reads=[pk, gtk], writes=[zk])
                    S.op("dve", nc.vector.scalar_tensor_tensor, zt[:], xt[:], ALPHA, zt[:], ALU.mult, ALU.add,
                         reads=[xk, zk], writes=[zk])
                    self.ln_rows(S, zt, zk, self.small, gb=(lng, lnb, lngk, lnbk), junk=junk)
                    S.dma("sp", self.xs[tok0:tok0 + 128, :], zt[:], reads=[zk], writes=["dram_xs"])
            S.barrier()

    def stage_moe(self, l, need_ctx):
        nc, S = self.nc, self.S
        last = l == DEPTH - 1
        tiles = list(range(0 if need_ctx else 2, NT))
        ntl = len(tiles)
        with ExitStack() as st0:
            def sbp(name, shape, dt=F32):
                return st0.enter_context(nc.sbuf_tensor(uq(name), shape, dt))
            SC = sbp("sb_SC", [128, NT, NE])
            M = sbp("sb_M", [128, NT, NE])
            IDXF = sbp("sb_IDXF", [128, NT, 8])
            DEST = sbp("sb_DEST", [128, NT, 8], I32)
            GATE = sbp("sb_GATE", [128, NT, 8])
            be_i = sbp("sb_bei", [128, NBIG], I32)
            start_bc = sbp("sb_start", [128, NE])
            with ExitStack() as st:
                zt = st.enter_context(nc.sbuf_tensor(uq("sb_zero"), [128, D], BF16))
                S.op("pool", nc.gpsimd.memset, zt[:], 0.0, writes=["zero"])
                for j in range(NBLK_R):
                    S.dma("sp" if j % 2 else "act", self.XS[j * 128:(j + 1) * 128, :], zt[:], reads=["zero"], writes=["dram_XS"])
                S.barrier()
            mstop = os.environ.get("MK_MOE_STOP", "")
            if mstop == "zero":
                return
            with ExitStack() as st:
                sc2 = [self.load_bc(st, "sc2l", self.modd[l, 0, 4 * D:5 * D]), self.load_bc(st, "sc2c", self.modd[l, 1, 4 * D:5 * D])]
                sh2 = [self.load_bc(st, "sh2l", self.modd[l, 0, 3 * D:4 * D]), self.load_bc(st, "sh2c", self.modd[l, 1, 3 * D:4 * D])]
                for (t_, k_) in sc2:
                    S.op("dve", nc.vector.tensor_scalar, t_[:], t_[:], 1.0, None, ALU.add, reads=[k_], writes=[k_])
                wr = st.enter_context(nc.sbuf_tensor(uq("sb_wr"), [128, KC, NE], F32))
                S.dma("sp", wr[:], self.inp["w_router"][l].rearrange("(kc p) e -> p kc e", p=128), writes=["wr"])
                rb = st.enter_context(nc.sbuf_tensor(uq("sb_rb"), [128, NE], F32))
                S.dma("sp", rb[:], self.inp["router_bias"][l, :].partition_broadcast(128), writes=["rb"])
                xp = Pool(nc, st, "rx", [128, D], F32, 2)
                hbp = Pool(nc, st, "rhb", [128, D], BF16, 2)
                hTp = Pool(nc, st, "rhT", [128, KC, 128], F32, 2)
                bip = Pool(nc, st, "rbi", [128, NE], F32, 2)
                v8p = Pool(nc, st, "rv8", [128, 8], F32, 2)
                i8p = Pool(nc, st, "ri8", [128, 8], U32, 2)
                for i in tiles:
                    m = 1 if i < 2 else 0
                    xt, xk = xp.get()
                    S.dma("sp", xt[:], self.xs[i * 128:(i + 1) * 128, :], reads=["dram_xs"], writes=[xk])
                    S.op("dve", nc.vector.tensor_tensor, xt[:], xt[:], sc2[m][0][:], ALU.mult, reads=[xk, sc2[m][1]], writes=[xk])
                    S.op("dve", nc.vector.tensor_tensor, xt[:], xt[:], sh2[m][0][:], ALU.add, reads=[xk, sh2[m][1]], writes=[xk])
                    hb, hbk = hbp.get()
                    S.op("act", nc.scalar.copy, hb[:], xt[:], reads=[xk], writes=[hbk])
                    S.dma("act", self.XS[BASE_SH + i * 128:BASE_SH + (i + 1) * 128, :], hb[:], reads=[hbk], writes=["dram_XS"])
                    hT, hTk = hTp.get()
                    for q in range(4):
                        ps, pk = self.psA.get()
                        for j in range(4):
                            kc = q * 4 + j
                            S.op("pe", nc.tensor.transpose, ps[:, j * 128:(j + 1) * 128], xt[:, kc * 128:(kc + 1) * 128], self.ident[:],
                                 reads=[xk, "ident"], writes=[pk])
                        S.op("act", nc.scalar.copy, hT[:, q * 4:q * 4 + 4, :].rearrange("p k t -> p (k t)"), ps[:, :], reads=[pk], writes=[hTk])
                    ps, pk = self.psB.get()
                    for kc in range(KC):
                        S.op("pe", nc.tensor.matmul, ps[:, :NE], hT[:, kc, :], wr[:, kc, :], start=(kc == 0), stop=(kc == KC - 1),
                             reads=[hTk, "wr"], writes=[pk])
                    S.op("act", nc.scalar.activation, SC[:, i, :], ps[:, :NE], AF.Sigmoid, reads=[pk], writes=["SC"])
                    bi_, bik = bip.get()
                    S.op("dve", nc.vector.tensor_tensor, bi_[:], SC[:, i, :], rb[:], ALU.add, reads=["SC", "rb"], writes=[bik])
                    v8, v8k = v8p.get()
                    S.op("dve", nc.vector.max, v8[:], bi_[:], reads=[bik], writes=[v8k])
                    i8, i8k = i8p.get()
                    S.op("dve", nc.vector.max_index, i8[:], v8[:], bi_[:], reads=[bik, v8k], writes=[i8k])
                    S.op("dve", nc.vector.tensor_copy, IDXF[:, i, :], i8[:], reads=[i8k], writes=["IDXF"])
                    S.op("dve", nc.vector.tensor_scalar, M[:, i, :], bi_[:], v8[:, 7:8], None, ALU.is_ge, reads=[bik, v8k], writes=["M"])
                S.barrier()
            if "MDBG" in self.dbg:
                md = self.dram("MDBG", [128, NT * NE])
                S.dma("sp", md, SC[:].rearrange("p t e -> p (t e)"), reads=["SC"], writes=["dram_MDBG"])
                md2 = self.dram("MDBG2", [128, NT * NE])
                S.dma("sp", md2, M[:].rearrange("p t e -> p (t e)"), reads=["M"], writes=["dram_MDBG2"])
                S.barrier()
            if mstop == "7a":
                return
            with ExitStack() as st:
                def sb(name, shape, dt=F32):
                    return st.enter_context(nc.sbuf_tensor(uq(name), shape, dt))
                sut = sb("sb_sut", [128, 128])
                iota = sb("sb_iota", [128, NE])
                blk = sb("sb_blk", [64, NBIG])
                S.dma("sp", sut[:], self.inp["sut"][:, :], writes=["sut"])
                S.dma("sp", iota[:], self.inp["iota64"][:, :], writes=["iota"])
                S.dma("sp", blk[:], self.inp["blk128"][:, :], writes=["blk"])
                pad = sb("sb_pad", [128, NE])
                padi = sb("sb_padi", [128, NE], I32)
                endb = sb("sb_end", [128, NE])
                padT = sb("sb_padT", [64, 128])
                endT = sb("sb_endT", [64, 128])
                cmp_ = sb("sb_cmp", [64, NBIG])
                bef = sb("sb_bef", [128, NBIG])
                pcol = sb("sb_pcol", [128, 1])
                S.dma("sp", pcol[:], self.inp["pcol"][:, :], writes=["pcol"])
                mcum = sb("sb_mcum", [128, NE])
                ps, pk = self.psA.get()
                for n_, i in enumerate(tiles):
                    S.op("pe", nc.tensor.matmul, ps[:, :NE], self.ones[:], M[:, i, :], start=(n_ == 0), stop=(n_ == ntl - 1),
                         reads=["ones", "M"], writes=[pk])
                S.op("dve", nc.vector.tensor_scalar, pad[:], ps[:, :NE], 1.0 / GRAN, 0.5 - 1.0 / (2 * GRAN), ALU.mult, ALU.add, reads=[pk], writes=["pad"])
                S.op("dve", nc.vector.tensor_copy, padi[:], pad[:], reads=["pad"], writes=["padi"])
                S.op("dve", nc.vector.tensor_copy, pad[:], padi[:], reads=["padi"], writes=["pad"])
                S.op("dve", nc.vector.tensor_scalar, pad[:], pad[:], float(GRAN), None, ALU.mult, reads=["pad"], writes=["pad"])
                ps, pk = self.psA.get()
                S.op("pe", nc.tensor.transpose, ps[:NE, :128], pad[:, :], self.ident[:], reads=["pad", "ident"], writes=[pk])
                S.op("act", nc.scalar.copy, padT[:], ps[:NE, :128], reads=[pk], writes=["padT"])
                ps, pk = self.psA.get()
                S.op("pe", nc.tensor.matmul, ps[:, :NE], padT[:, :], sut[0:NE, 0:NE], start=True, stop=True, reads=["padT", "sut"], writes=[pk])
                S.op("act", nc.scalar.copy, start_bc[:], ps[:, :NE], reads=[pk], writes=["start"])
                S.op("dve", nc.vector.tensor_tensor, endb[:], start_bc[:], pad[:], ALU.add, reads=["start", "pad"], writes=["endb"])
                ps, pk = self.psA.get()
                S.op("pe", nc.tensor.transpose, ps[:NE, :128], endb[:, :], self.ident[:], reads=["endb", "ident"], writes=[pk])
                S.op("act", nc.scalar.copy, endT[:], ps[:NE, :128], reads=[pk], writes=["endT"])
                S.op("dve", nc.vector.tensor_scalar, cmp_[:], blk[:], endT[:, 0:1], None, ALU.is_ge, reads=["blk", "endT"], writes=["cmp"])
                ps, pk = self.psA.get()
                S.op("pe", nc.tensor.matmul, ps[:, :NBIG], self.ones[0:NE, :], cmp_[:, :], start=True, stop=True,
                     reads=["ones", "cmp"], writes=[pk])
                S.op("dve", nc.vector.tensor_scalar, bef[:], ps[:, :NBIG], float(NE - 1), None, ALU.min, reads=[pk], writes=["bef"])
                S.op("dve", nc.vector.tensor_scalar, bef[:], bef[:], 128.0, pcol[:, 0:1], ALU.mult, ALU.add, reads=["bef", "pcol"], writes=["bef"])
                S.op("dve", nc.vector.tensor_scalar, bef[:], bef[:], float(l * NE * 128), None, ALU.add, reads=["bef"], writes=["bef"])
                S.op("dve", nc.vector.tensor_copy, be_i[:], bef[:], reads=["bef"], writes=["be"])
                S.op("pool", nc.gpsimd.memset, mcum[:], 0.0, writes=["mcum"])
                dp = Pool(nc, st, "sdest", [128, NE], F32, 2)
                ohp = Pool(nc, st, "soh", [128, NE], F32, 3)
                jp = Pool(nc, st, "sjunk", [128, NE], F32, 2)
                d8p = Pool(nc, st, "sd8", [128, 8], F32, 2)
                g8p = Pool(nc, st, "sg8", [128, 8], F32, 2)
                hbp = Pool(nc, st, "shb", [128, D], BF16, 3)
                for i in tiles:
                    ps, pk = self.psA.get()
                    S.op("pe", nc.tensor.matmul, ps[:, :NE], sut[:], M[:, i, :], start=True, stop=False, reads=["sut", "M"], writes=[pk])
                    S.op("pe", nc.tensor.matmul, ps[:, :NE], self.ones[:], mcum[:], start=False, stop=True, reads=["ones", "mcum"], writes=[pk])
                    dst, dk = dp.get()
                    S.op("dve", nc.vector.tensor_tensor, dst[:], ps[:, :NE], start_bc[:], ALU.add, reads=[pk, "start"], writes=[dk])
                    S.op("pool", nc.gpsimd.tensor_tensor, mcum[:], mcum[:], M[:, i, :], ALU.add, reads=["mcum", "M"], writes=["mcum"])
                    d8, d8k = d8p.get()
                    g8, g8k = g8p.get()
                    for k in range(8):
                        oh, ohk = ohp.get()
                        S.op("dve", nc.vector.tensor_scalar, oh[:], iota[:], IDXF[:, i, k:k + 1], None, ALU.is_equal,
                             reads=["iota", "IDXF"], writes=[ohk])
                        jk, jkk = jp.get()
                        S.op("dve", nc.vector.scalar_tensor_tensor, jk[:], oh[:], 1.0, dst[:], ALU.mult, ALU.mult, accum_out=d8[:, k:k + 1],
                             reads=[ohk, dk], writes=[jkk, d8k])
                        jk, jkk = jp.get()
                        S.op("dve", nc.vector.scalar_tensor_tensor, jk[:], oh[:], 1.0, SC[:, i, :], ALU.mult, ALU.mult, accum_out=g8[:, k:k + 1],
                             reads=[ohk, "SC"], writes=[jkk, g8k])
                    S.op("dve", nc.vector.tensor_copy, DEST[:, i, :], d8[:], reads=[d8k], writes=["DEST"])
                    sm, smk = self.small.get()
                    S.op("dve", nc.vector.reduce_sum, sm[:, 0:1], g8[:], AX.X, reads=[g8k], writes=[smk])
                    S.op("dve", nc.vector.reciprocal, sm[:, 1:2], sm[:, 0:1], reads=[smk], writes=[smk])
                    S.op("dve", nc.vector.tensor_scalar, GATE[:, i, :], g8[:], sm[:, 1:2], 2.5, ALU.mult, ALU.mult,
                         reads=[g8k, smk], writes=["GATE"])
                    hb, hbk = hbp.get()
                    S.dma("sp", hb[:], self.XS[BASE_SH + i * 128:BASE_SH + (i + 1) * 128, :], reads=["dram_XS"], writes=[hbk])
                    for k in range(8):
                        S.dma("pool", None, None, reads=[hbk, "DEST"], writes=["dram_XS"],
                              indirect=(lambda hb=hb, i=i, k=k: nc.gpsimd.indirect_dma_start(
                                  out=self.XS[:, :], out_offset=bass.IndirectOffsetOnAxis(ap=DEST[:, i, k:k + 1], axis=0),
                                  in_=hb[:, :], in_offset=None)))
                S.barrier()
            if "MDBG3" in self.dbg:
                md3 = self.dram("MDBG3", [128, NT * 8], I32)
                S.dma("sp", md3, DEST[:].rearrange("p t e -> p (t e)"), reads=["DEST"], writes=["dram_MDBG3"])
                md4 = self.dram("MDBG4", [128, NT * 8])
                S.dma("sp", md4, GATE[:].rearrange("p t e -> p (t e)"), reads=["GATE"], writes=["dram_MDBG4"])
                md5 = self.dram("MDBG5", [128, NBIG], I32)
                S.dma("sp", md5, be_i[:], reads=["be"], writes=["dram_MDBG5"])
                S.barrier()
            if mstop == "7c":
                return
            with ExitStack() as st:
                identb = st.enter_context(nc.sbuf_tensor(uq("sb_identb"), [128, 128], BF16))
                S.dma("pool", identb[:], self.inp["ident"][:, :], writes=["identb"])
                w1p = Pool(nc, st, "ew1", [128, KC, EFF], BF16, 2)
                w3p = Pool(nc, st, "ew3", [128, KC, EFF], BF16, 2)
                w2p = Pool(nc, st, "ew2", [128, 4, D], BF16, 2)
                xgp = Pool(nc, st, "exg", [128, D], BF16, 2)
                xTp = Pool(nc, st, "exT", [128, KC, 128], BF16, 2)
                s1p = Pool(nc, st, "es1", [128, EFF], F32, 2)
                gbp = Pool(nc, st, "egb", [128, EFF], BF16, 2)
                gTp = Pool(nc, st, "egT", [128, 4, 128], BF16, 2)
                yp = Pool(nc, st, "ey", [128, D], BF16, 2)

                def ffn_block(row0, w1t, w3t, w2t, wkeys):
                    xg, xgk = xgp.get()
                    S.dma("sp", xg[:], self.XS[row0:row0 + 128, :], reads=["dram_XS"], writes=[xgk])
                    xT, xTk = xTp.get()
                    for hlf in range(2):
                        ps, pk = self.psB.get()
                        pb = ps[:, :].bitcast(BF16)
                        for j in range(8):
                            kc = hlf * 8 + j
                            S.op("pe", nc.tensor.transpose, pb[:, j * 128:(j + 1) * 128], xg[:, kc * 128:(kc + 1) * 128], identb[:],
                                 reads=[xgk, "identb"], writes=[pk])
                        S.op("act", nc.scalar.copy, xT[:, hlf * 8:hlf * 8 + 8, :].rearrange("p k t -> p (k t)"), pb[:, :], reads=[pk], writes=[xTk])
                    p1, p1k = self.psA.get()
                    p3, p3k = self.psA.get()
                    for kc in range(KC):
                        S.op("pe", nc.tensor.matmul, p1[:, :], xT[:, kc, :], w1t[:, kc, :], start=(kc == 0), stop=(kc == KC - 1),
                             reads=[xTk, wkeys[0]], writes=[p1k])
                    for kc in range(KC):
                        S.op("pe", nc.tensor.matmul, p3[:, :], xT[:, kc, :], w3t[:, kc, :], start=(kc == 0), stop=(kc == KC - 1),
                             reads=[xTk, wkeys[1]], writes=[p3k])
                    s1, s1k = s1p.get()
                    S.op("act", nc.scalar.activation, s1[:], p1[:, :], AF.Silu, reads=[p1k], writes=[s1k])
                    gb, gbk = gbp.get()
                    S.op("dve", nc.vector.tensor_tensor, gb[:], p3[:, :], s1[:], ALU.mult, reads=[p3k, s1k], writes=[gbk])
                    ps, pk = self.psB.get()
                    pb = ps[:, :].bitcast(BF16)
                    for fc in range(4):
                        S.op("pe", nc.tensor.transpose, pb[:, fc * 128:(fc + 1) * 128], gb[:, fc * 128:(fc + 1) * 128], identb[:],
                             reads=[gbk, "identb"], writes=[pk])
                    gT, gTk = gTp.get()
                    S.op("act", nc.scalar.copy, gT[:].rearrange("p k t -> p (k t)"), pb[:, :512], reads=[pk], writes=[gTk])
                    y, yk = yp.get()
                    for q in range(4):
                        ps, pk = self.psA.get()
                        for fc in range(4):
                            S.op("pe", nc.tensor.matmul, ps[:, :], gT[:, fc, :], w2t[:, fc, q * 512:(q + 1) * 512], start=(fc == 0), stop=(fc == 3),
                                 reads=[gTk, wkeys[2]], writes=[pk])
                        if q % 2 == 0:
                            S.op("dve", nc.vector.tensor_copy, y[:, q * 512:(q + 1) * 512], ps[:, :], reads=[pk], writes=[yk])
                        else:
                            S.op("act", nc.scalar.copy, y[:, q * 512:(q + 1) * 512], ps[:, :], reads=[pk], writes=[yk])
                    S.dma("act", self.YS[row0:row0 + 128, :], y[:], reads=[yk], writes=["dram_YS"])

                nblk = int(os.environ.get("MK_MOE_NBLK", str(NBIG)))
                for j in range(nblk):
                    w1t, w1k = w1p.get()
                    w3t, w3k = w3p.get()
                    w2t, w2k = w2p.get()
                    for (wt_, wk_, nm) in ((w1t, w1k, "w1r"), (w3t, w3k, "w3r"), (w2t, w2k, "w2r")):
                        oap = wt_[:].rearrange("p a n -> p (a n)")
                        S.dma("pool", None, None, reads=["be"], writes=[wk_],
                              indirect=(lambda oap=oap, nm=nm, j=j: nc.gpsimd.indirect_dma_start(
                                  out=oap, out_offset=None, in_=self.inp[nm][:, :],
                                  in_offset=bass.IndirectOffsetOnAxis(ap=be_i[:, j:j + 1], axis=0))))
                    for sub in range(GRAN // 128):
                        ffn_block(j * GRAN + sub * 128, w1t, w3t, w2t, (w1k, w3k, w2k))
                w1t, w1k = w1p.get()
                w3t, w3k = w3p.get()
                w2t, w2k = w2p.get()
                S.dma("pool", w1t[:], self.inp["ws1"][l].rearrange("(kc p) n -> p kc n", p=128), writes=[w1k])
                S.dma("pool", w3t[:], self.inp["ws3"][l].rearrange("(kc p) n -> p kc n", p=128), writes=[w3k])
                S.dma("pool", w2t[:], self.inp["ws2"][l].rearrange("(fc p) n -> p fc n", p=128), writes=[w2k])
                for i in tiles:
                    ffn_block(BASE_SH + i * 128, w1t, w3t, w2t, (w1k, w3k, w2k))
                S.barrier()
            if mstop == "7d":
                return
            with ExitStack() as st:
                gt = [self.load_bc(st, "gt2l", self.modd[l, 0, 5 * D:6 * D]), self.load_bc(st, "gt2c", self.modd[l, 1, 5 * D:6 * D])]
                lng, lngk = self.load_bc(st, "ln2g", self.inp["ln2_g"][l, :])
                lnb, lnbk = self.load_bc(st, "ln2b", self.inp["ln2_b"][l, :])
                accp = Pool(nc, st, "cacc", [128, D], F32, 2)
                a0p = Pool(nc, st, "ca0", [128, D], BF16, 2)
                ykp = Pool(nc, st, "cyk", [128, D], BF16, 3)
                xp = Pool(nc, st, "cx", [128, D], F32, 2)
                junk = Pool(nc, st, "cjunk", [128, D], F32, 1)
                for i in tiles:
                    m = 1 if i < 2 else 0
                    acc, acck = accp.get()
                    a0, a0k = a0p.get()
                    S.dma("sp", a0[:], self.YS[BASE_SH + i * 128:BASE_SH + (i + 1) * 128, :], reads=["dram_YS"], writes=[a0k])
                    S.op("act", nc.scalar.copy, acc[:], a0[:], reads=[a0k], writes=[acck])
                    for k in range(8):
                        yk_, ykk = ykp.get()
                        S.dma("pool", None, None, reads=["dram_YS", "DEST"], writes=[ykk],
                              indirect=(lambda yk_=yk_, i=i, k=k: nc.gpsimd.indirect_dma_start(
                                  out=yk_[:, :], out_offset=None, in_=self.YS[:, :],
                                  in_offset=bass.IndirectOffsetOnAxis(ap=DEST[:, i, k:k + 1], axis=0))))
                        S.op("dve", nc.vector.scalar_tensor_tensor, acc[:], yk_[:], GATE[:, i, k:k + 1], acc[:], ALU.mult, ALU.add,
                             reads=[ykk, "GATE", acck], writes=[acck])
                    xt, xk = xp.get()
                    S.dma("act", xt[:], self.xs[i * 128:(i + 1) * 128, :], reads=["dram_xs"], writes=[xk])
                    S.op("dve", nc.vector.tensor_tensor, acc[:], acc[:], gt[m][0][:], ALU.mult, reads=[acck, gt[m][1]], writes=[acck])
                    S.op("dve", nc.vector.scalar_tensor_tensor, acc[:], xt[:], ALPHA, acc[:], ALU.mult, ALU.add,
                         reads=[xk, acck], writes=[acck])
                    self.ln_rows(S, acc, acck, self.small, gb=(lng, lnb, lngk, lnbk), junk=junk)
                    if last:
                        S.dma("sp", self.out[(i - 2) * 128:(i - 1) * 128, :], acc[:], reads=[acck], writes=["dram_out"])
                    else:
                        S.dma("sp", self.xs[i * 128:(i + 1) * 128, :], acc[:], reads=[acck], writes=["dram_xs"])
                S.barrier()


_CACHE = {}


def _layout_inputs(inputs, core):
    m = {}
    x = np.asarray(inputs["x"], dtype=np.float32)
    ctx = np.asarray(inputs["ctx"], dtype=np.float32)
    c = np.asarray(inputs["c"], dtype=np.float32)
    cc = np.asarray(inputs["c_ctx"], dtype=np.float32)
    xs, cs = [], []
    for j in range(NB):
        b = (core * NB + j) % 4
        xs.append(np.concatenate([ctx[b], x[b]], axis=0))
        cs.append(np.stack([c[b].reshape(KC, 128).T, cc.reshape(KC, 128).T], axis=-1))
    m["xin"] = np.ascontiguousarray(np.stack(xs, axis=0))
    m["cT"] = np.ascontiguousarray(np.stack(cs, axis=0))
    return m


def kernel(**inputs):
    dbg = tuple(x for x in os.environ.get("MK_DBG", "").split(",") if x)
    stop = os.environ.get("MK_STOP", "all")
    ncores = int(os.environ.get("MK_CORES", str(4 // NB)))
    bld = Builder(dbg)
    nc = bld.build(stop)
    consts = host_consts()
    shared = {}
    for k in bld.inp.used:
        if k not in W_SHAPES:
            continue
        if k in ("w1r", "w3r"):
            w = np.asarray(inputs["w1"] if k == "w1r" else inputs["w3"], dtype=np.float32)
            w = w.reshape(DEPTH, NE, KC, 128, EFF).transpose(0, 1, 3, 2, 4)
            shared[k] = np.ascontiguousarray(w).reshape(DEPTH * NE * 128, 8192)
        elif k == "w2r":
            w = np.asarray(inputs["w2"], dtype=np.float32)
            w = w.reshape(DEPTH, NE, 4, 128, D).transpose(0, 1, 3, 2, 4)
            shared[k] = np.ascontiguousarray(w).reshape(DEPTH * NE * 128, 8192)
        else:
            shared[k] = np.ascontiguousarray(np.asarray(inputs[k], dtype=np.float32))
    consts = {k: v for k, v in consts.items() if k in bld.inp.used}
    in_maps = []
    for core in range(ncores):
        m = _layout_inputs(inputs, core)
        m.update(consts)
        m.update(shared)
        in_maps.append(m)
    res = run_bass_kernel_spmd(nc, in_maps, core_ids=list(range(ncores)))
    kernel.last = res
    out = np.concatenate([np.asarray(res.results[c]["out"]) for c in range(ncores)], axis=0)
    return np.ascontiguousarray(out[:4]).astype(np.float32)
```

```python
import os
import math
from contextlib import ExitStack

import numpy as np
import concourse.bass as bass
import concourse.mybir as mybir
from concourse.bass_utils import run_bass_kernel_spmd

F32 = mybir.dt.float32
BF16 = mybir.dt.bfloat16
I32 = mybir.dt.int32
U32 = mybir.dt.uint32
AF = mybir.ActivationFunctionType
ALU = mybir.AluOpType
AX = mybir.AxisListType

D = 2048
KC = 16
NCTX = 256
SEQ = 4096
T = NCTX + SEQ
NT = T // 128
DEPTH = 2
HD = 128
IN_W = 12320
MODW = 6 * D
NE = 64
TOPK = 8
EFF = 512
ALPHA = (2.0 * DEPTH) ** 0.25
LN_EPS = 1e-5
RMS_EPS = 1e-6
O_AQ, O_AK, O_AV = 0, 1024, 1280
O_BQ, O_BK, O_BV = 1536, 2560, 2816
O_CQ, O_CK, O_CV, O_CZ = 3072, 3584, 4096, 5120
O_CA, O_CB = 6144, 6160
O_G = 6176
NB = int(os.environ.get("MK_NB", "1"))
GRAN = 256
NBIG = (T * TOPK) // GRAN + NE
NBLK_R = NBIG * (GRAN // 128)
BASE_SH = NBLK_R * 128
NSLOT = BASE_SH + T


class Sched:
    def __init__(self, nc, stack, n_dma_sems=(8, 4, 8)):
        self.nc = nc
        self.eng = {"pe": nc.tensor, "dve": nc.vector, "act": nc.scalar,
                    "pool": nc.gpsimd, "sp": nc.sync}
        self.sem = {}
        self.cnt = {}
        for e in ("pe", "dve", "act", "pool"):
            self.sem[e] = stack.enter_context(nc.semaphore("s_" + e))
            self.cnt[e] = 0
        self.dsem = {}
        self.dcnt = {}
        for q, n in zip(("sp", "act", "pool"), n_dma_sems):
            self.dsem[q] = [stack.enter_context(nc.semaphore("d_%s%d" % (q, i))) for i in range(n)]
            self.dcnt[q] = 0
        self.waited = {}
        self.lastw = {}
        self.readers = {}
        self.n_inst = 0
        self.nbar = 0
        self.sem_arrive = stack.enter_context(nc.semaphore("s_arrive"))
        self.sem_release = stack.enter_context(nc.semaphore("s_release"))

    def _wait(self, e, ev):
        if ev is None:
            return
        sem, name, val, src = ev
        if src == e and e == "pe":
            return
        k = (e, name)
        if self.waited.get(k, 0) >= val:
            return
        self.eng[e].wait_ge(sem, val)
        self.waited[k] = val
        self.n_inst += 1

    def _deps(self, e, reads, writes):
        for k in reads:
            self._wait(e, self.lastw.get(k))
        for k in writes:
            self._wait(e, self.lastw.get(k))
            for ev in self.readers.get(k, ()):
                self._wait(e, ev)

    def _commit(self, ev, reads, writes):
        for k in writes:
            self.lastw[k] = ev
            self.readers[k] = []
        for k in reads:
            lst = self.readers.setdefault(k, [])
            lst[:] = [x for x in lst if x[1] != ev[1]]
            lst.append(ev)

    def op(self, e, fn, *args, reads=(), writes=(), **kw):
        self._deps(e, reads, writes)
        ins = fn(*args, **kw)
        self.cnt[e] += 1
        ins.then_inc(self.sem[e], 1)
        ev = (self.sem[e], "s_" + e, self.cnt[e], e)
        self._commit(ev, reads, writes)
        self.n_inst += 1
        return ev

    def dma(self, q, out, in_, reads=(), writes=(), indirect=None, **kw):
        sems = self.dsem[q]
        i = self.dcnt[q]
        r = i % len(sems)
        m = i // len(sems)
        sem = sems[r]
        name = "d_%s%d" % (q, r)
        if m > 0:
            k = (q, name)
            if self.waited.get(k, 0) < 16 * m:
                self.eng[q].wait_ge(sem, 16 * m)
                self.waited[k] = 16 * m
        self._deps(q, reads, writes)
        if indirect is None:
            ins = self.eng[q].dma_start(out=out, in_=in_, **kw)
        else:
            ins = indirect()
        ins.then_inc(sem, 16)
        self.dcnt[q] += 1
        ev = (sem, name, 16 * (m + 1), "dma_" + q)
        self._commit(ev, reads, writes)
        self.n_inst += 1
        return ev

    def barrier(self):
        evs = []
        for e in ("pe", "dve", "act", "pool"):
            if self.cnt[e] > 0:
                evs.append((self.sem[e], "s_" + e, self.cnt[e], e))
        for q in ("sp", "act", "pool"):
            n = len(self.dsem[q])
            for r in range(n):
                issued = (self.dcnt[q] - r + n - 1) // n
                if issued > 0:
                    evs.append((self.dsem[q][r], "d_%s%d" % (q, r), 16 * issued, "dma_" + q))
        engs = ("pe", "dve", "act", "pool", "sp")
        for e in engs:
            for ev in evs:
                sem, name, val, src = ev
                k = (e, name)
                if self.waited.get(k, 0) >= val:
                    continue
                self.eng[e].wait_ge(sem, val)
                self.waited[k] = val
                self.n_inst += 1

    def maybe_barrier(self):
        return

    def finish(self, e, keys):
        for k in keys:
            self._wait(e, self.lastw.get(k))


_UQ = [0]


def uq(name):
    _UQ[0] += 1
    return "%s_u%d" % (name, _UQ[0])


class Pool:
    def __init__(self, nc, stack, name, shape, dtype, n, psum=False):
        self.bufs = []
        name = uq(name)
        for i in range(n):
            nm = "%s_%d" % (name, i)
            if psum:
                t = stack.enter_context(nc.psum_tensor(nm, shape, dtype))
            else:
                t = stack.enter_context(nc.sbuf_tensor(nm, shape, dtype))
            self.bufs.append((t, nm))
        self.i = 0

    def get(self):
        b = self.bufs[self.i % len(self.bufs)]
        self.i += 1
        return b


def host_consts():
    c = {}
    c["ident"] = np.eye(128, dtype=np.float32)
    c["ones"] = np.ones((128, 128), dtype=np.float32)
    rows = SEQ // 64
    row = np.repeat(np.arange(rows, dtype=np.float32), 64)
    col = np.tile(np.arange(64, dtype=np.float32), rows)
    inv_freq = (np.float32(10000.0) ** (-np.arange(0, 64, 2, dtype=np.float32) / np.float32(64))).astype(np.float32)
    ang_r = row[:, None] * inv_freq[None, :]
    ang_c = col[:, None] * inv_freq[None, :]
    ang = np.concatenate([ang_r, ang_r, ang_c, ang_c], axis=-1).astype(np.float32)
    c["cosT"] = np.ascontiguousarray(np.cos(ang).astype(np.float32).T)
    c["sinT"] = np.ascontiguousarray(np.sin(ang).astype(np.float32).T)
    rot = np.zeros((128, 128), np.float32)
    for d in range(128):
        if d % 64 < 32:
            rot[d + 32, d] = -1.0
        else:
            rot[d - 32, d] = 1.0
    c["rotm"] = rot
    wm = np.zeros((128, 6, 512), np.float32)
    kk = np.arange(128)[:, None]
    qq = np.arange(128)[None, :]
    for r in range(-1, 5):
        for j in range(4):
            dlt = r - j
            if dlt == 0:
                m = np.ones((128, 128), np.float32)
            elif dlt == -1:
                m = (qq <= kk).astype(np.float32)
            elif dlt == 1:
                m = (kk <= qq).astype(np.float32)
            else:
                m = np.zeros((128, 128), np.float32)
            wm[:, r + 1, j * 128:(j + 1) * 128] = m
    c["wmask"] = wm
    ii = np.arange(128)[:, None]
    jj = np.arange(128)[None, :]
    c["tri_f"] = (ii <= jj).astype(np.float32)
    c["tri_b"] = (ii >= jj).astype(np.float32)
    c["negm_f"] = np.where(jj <= ii, 0.0, -30000.0).astype(np.float32)
    c["negm_b"] = np.where(jj >= ii, 0.0, -30000.0).astype(np.float32)
    c["offdiag"] = (1.0 - np.eye(128)).astype(np.float32)
    c["sut"] = (ii < jj).astype(np.float32)
    c["iota64"] = np.tile(np.arange(64, dtype=np.float32)[None, :], (128, 1))
    c["blk128"] = np.tile((float(GRAN) * np.arange(NBIG, dtype=np.float32))[None, :], (64, 1))
    c["pcol"] = np.arange(128, dtype=np.float32)[:, None].copy()
    return c


CONST_SHAPES = {"ident": [128, 128], "ones": [128, 128], "cosT": [128, SEQ], "sinT": [128, SEQ],
                "rotm": [128, 128], "wmask": [128, 6, 512], "tri_f": [128, 128], "tri_b": [128, 128],
                "negm_f": [128, 128], "negm_b": [128, 128], "offdiag": [128, 128],
                "sut": [128, 128], "iota64": [128, 64], "blk128": [64, NBIG], "pcol": [128, 1]}

W_SHAPES = {
    "w_mod": [DEPTH, D, MODW], "b_mod": [DEPTH, MODW], "w_in": [DEPTH, D, IN_W],
    "q_norm_a": [DEPTH, HD], "k_norm_a": [DEPTH, HD], "sink_b": [DEPTH, 8],
    "conv_c": [DEPTH, 5, 2048], "a_log_c": [DEPTH, 2, 8], "dt_bias_c": [DEPTH, 2, 8],
    "norm_c": [DEPTH, HD], "w_br_a": [DEPTH, 1024, D], "w_br_b": [DEPTH, 1024, D],
    "w_br_c": [DEPTH, 1024, D], "w_out": [DEPTH, D, D], "ln1_g": [DEPTH, D], "ln1_b": [DEPTH, D],
    "w_router": [DEPTH, D, NE], "router_bias": [DEPTH, NE],
    "w1r": [DEPTH * NE * 128, 8192], "w3r": [DEPTH * NE * 128, 8192], "w2r": [DEPTH * NE * 128, 8192],
    "ws1": [DEPTH, D, EFF], "ws3": [DEPTH, D, EFF], "ws2": [DEPTH, EFF, D],
    "ln2_g": [DEPTH, D], "ln2_b": [DEPTH, D],
}


class LazyIn(dict):
    def __init__(self, nc, base):
        super().__init__(base)
        self.nc = nc
        self.used = []

    def __missing__(self, k):
        shp = CONST_SHAPES[k] if k in CONST_SHAPES else W_SHAPES[k]
        ap = self.nc.dram_tensor(k, shp, F32, kind="ExternalInput").ap()
        self[k] = ap
        self.used.append(k)
        return ap


class Builder:
    def __init__(self, dbg=()):
        self.dbg = set(dbg)
        self.nc = bass.Bass("TRN2", target_bir_lowering=False)
        self.outs = []

    def dram(self, name, shape, dtype=F32):
        kind = "ExternalOutput" if name in self.dbg else "Internal"
        if name in self.dbg:
            self.outs.append(name)
        return self.nc.dram_tensor(name, shape, dtype, kind=kind).ap()

    def ln_rows(self, S, xt, key, small, n=128, gb=None, junk=None):
        nc = self.nc
        st, stk = small.get()
        sq, sqk = (junk or self.junk).get()
        S.op("dve", nc.vector.reduce_sum, st[:n, 0:1], xt[:n, :], AX.X, reads=[key], writes=[stk])
        S.op("dve", nc.vector.tensor_scalar, st[:n, 1:2], st[:n, 0:1], -1.0 / D, None, ALU.mult,
             reads=[stk], writes=[stk])
        S.op("dve", nc.vector.tensor_scalar, xt[:n, :], xt[:n, :], st[:n, 1:2], None, ALU.add,
             reads=[stk, key], writes=[key])
        S.op("act", nc.scalar.activation, sq[:n, :], xt[:n, :], AF.Square, accum_out=st[:n, 2:3],
             reads=[key], writes=[sqk, stk])
        S.op("act", nc.scalar.activation, st[:n, 3:4], st[:n, 2:3], AF.Sqrt, bias=LN_EPS, scale=1.0 / D,
             reads=[stk], writes=[stk])
        S.op("dve", nc.vector.reciprocal, st[:n, 4:5], st[:n, 3:4], reads=[stk], writes=[stk])
        S.op("dve", nc.vector.tensor_scalar, xt[:n, :], xt[:n, :], st[:n, 4:5], None, ALU.mult,
             reads=[stk, key], writes=[key])
        if gb is not None:
            g, b, gk, bk = gb
            S.op("dve", nc.vector.tensor_tensor, xt[:n, :], xt[:n, :], g[:n, :], ALU.mult,
                 reads=[key, gk], writes=[key])
            S.op("dve", nc.vector.tensor_tensor, xt[:n, :], xt[:n, :], b[:n, :], ALU.add,
                 reads=[key, bk], writes=[key])

    def build(self, stop_after="all"):
        nc = self.nc
        self.inp = {}
        self.inp["xin"] = nc.dram_tensor("xin", [NB, T, D], F32, kind="ExternalInput").ap()
        self.inp["cT"] = nc.dram_tensor("cT", [NB, 128, KC, 2], F32, kind="ExternalInput").ap()
        self.inp = LazyIn(nc, self.inp)
        self.out_all = nc.dram_tensor("out", [NB, SEQ, D], F32, kind="ExternalOutput").ap()
        self.bi = 0
        self.out = self.out_all[0]
        self.xs = self.dram("xs", [T, D])
        self.modd = self.dram("modd", [DEPTH, 2, MODW])
        self.PT = self.dram("PT", [IN_W, T])
        self.QK = self.dram("QK", [20, 128, T], BF16)
        self.OT = self.dram("OT", [3072, T], BF16)
        self.CQT = self.dram("CQT", [4, 128, T])
        self.CKT = self.dram("CKT", [4, 128, T])
        self.CKtm = self.dram("CKtm", [4, T, 128])
        self.CVtm = self.dram("CVtm", [8, T, 128])
        self.OC = [self.dram("OC0", [T, 1024]), self.dram("OC1", [T, 1024])]
        self.YP = self.dram("YP", [D, T], BF16)
        self.XS = self.dram("XSORT", [NSLOT, D], BF16)
        self.YS = self.dram("YSORT", [NSLOT, D], BF16)

        with ExitStack() as st:
            S = Sched(nc, st)
            self.S = S
            self.st = st
            self.psA = Pool(nc, st, "psA", [128, 512], F32, 6, psum=True)
            self.psB = Pool(nc, st, "psB", [128, 512], F32, 2, psum=True)
            self.small = Pool(nc, st, "small", [128, 8], F32, 4)
            self.ident = st.enter_context(nc.sbuf_tensor(uq("sb_ident"), [128, 128], F32))
            self.ones = st.enter_context(nc.sbuf_tensor(uq("sb_ones"), [128, 128], F32))
            self.onesb = st.enter_context(nc.sbuf_tensor(uq("sb_onesb"), [128, 128], BF16))
            S.dma("sp", self.ident[:], self.inp["ident"][:, :], writes=["ident"])
            S.dma("sp", self.ones[:], self.inp["ones"][:, :], writes=["ones"])
            S.dma("pool", self.onesb[:], self.inp["ones"][:, :], writes=["onesb"])
            self.modc = st.enter_context(nc.sbuf_tensor(uq("sb_modc"), [128, 96, 2], F32))

            if stop_after.startswith("only_"):
                getattr(self, "stage_" + stop_after[5:])(0, True)
                S.finish("sp", ["dram_" + n for n in self.outs] + ["dram_out"])
                return nc
            for bi in range(NB):
                self.bi = bi
                self.out = self.out_all[bi]
                self.stage_entry_ln()
                for l in range(DEPTH):
                    if stop_after == "ln":
                        break
                    self.stage_mod(l)
                    if stop_after == "mod":
                        break
                    need_ctx = l < DEPTH - 1
                    self.stage_inproj(l)
                    if stop_after == "inproj":
                        break
                    self.stage_qkprep(l)
                    if stop_after == "qkprep":
                        break
                    self.stage_attn(l, "A", need_ctx)
                    self.stage_attn(l, "B", need_ctx)
                    if stop_after == "attn":
                        break
                    self.stage_delta_prep(l)
                    if stop_after == "dprep":
                        break
                    self.stage_delta_scan(l, need_ctx)
                    if stop_after == "dscan":
                        break
                    self.stage_delta_out(l, need_ctx)
                    if stop_after == "delta":
                        break
                    self.stage_merge(l, need_ctx)
                    if stop_after == "merge":
                        break
                    self.stage_wout_ln(l, need_ctx)
                    if stop_after == "ln1":
                        break
                    self.stage_moe(l, need_ctx)
                    if stop_after == "moe%d" % l:
                        break
            S.finish("sp", ["dram_" + n for n in self.outs] + ["dram_out"])
        return nc

    def stage_entry_ln(self):
        nc, S = self.nc, self.S
        with ExitStack() as st:
            xp = Pool(nc, st, "elx", [128, D], F32, 3)
            self.junk = Pool(nc, st, "junk", [128, D], F32, 1)
            for i in range(NT):
                xt, k = xp.get()
                S.dma("sp", xt[:], self.inp["xin"][self.bi, i * 128:(i + 1) * 128, :], writes=[k])
                self.ln_rows(S, xt, k, self.small)
                S.dma("act", self.xs[i * 128:(i + 1) * 128, :], xt[:], reads=[k], writes=["dram_xs"])
            S.barrier()

    def stage_mod(self, l):
        nc, S = self.nc, self.S
        with ExitStack() as st:
            cT = st.enter_context(nc.sbuf_tensor(uq("cTs"), [128, KC, 2], F32))
            sg = st.enter_context(nc.sbuf_tensor(uq("cTg"), [128, KC, 2], F32))
            S.dma("sp", cT[:], self.inp["cT"][self.bi], writes=["cTs"])
            S.op("act", nc.scalar.activation, sg[:], cT[:], AF.Sigmoid, reads=["cTs"], writes=["cTg"])
            S.op("dve", nc.vector.tensor_tensor, cT[:], cT[:], sg[:], ALU.mult, reads=["cTs", "cTg"], writes=["cTs"])
            wp = Pool(nc, st, "wmod", [128, KC, 512], F32, 2)
            bp = Pool(nc, st, "bmod", [2, 512], F32, 2)
            op_ = Pool(nc, st, "omod", [2, 512], F32, 2)
            wsrc = self.inp["w_mod"]
            for nb in range(MODW // 512):
                wt, wk = wp.get()
                S.dma("sp" if nb % 2 == 0 else "act", wt[:],
                      wsrc[l, :, nb * 512:(nb + 1) * 512].rearrange("(kc p) n -> p kc n", p=128), writes=[wk])
                bt, bk = bp.get()
                for m in range(2):
                    S.dma("sp", bt[m:m + 1, :], self.inp["b_mod"][l:l + 1, nb * 512:(nb + 1) * 512], writes=[bk])
                ps, pk = self.psA.get()
                for kc in range(KC):
                    S.op("pe", nc.tensor.matmul, ps[0:2, :], cT[:, kc, :], wt[:, kc, :],
                         start=(kc == 0), stop=(kc == KC - 1), reads=["cTs", wk], writes=[pk])
                ot, ok = op_.get()
                S.op("dve", nc.vector.tensor_tensor, ot[:], ps[0:2, :], bt[:], ALU.add, reads=[pk, bk], writes=[ok])
                S.dma("sp", self.modd[l, :, nb * 512:(nb + 1) * 512], ot[:], reads=[ok], writes=["dram_modd"])
            for m in range(2):
                S.dma("sp", self.modc[:, :, m], self.modd[l, m, :].rearrange("(j p) -> p j", p=128),
                      reads=["dram_modd"], writes=["modc"], allow_slow_non_contiguous=True)
            for j0 in (16, 64):
                S.op("dve", nc.vector.tensor_scalar, self.modc[:, j0:j0 + 16, :], self.modc[:, j0:j0 + 16, :],
                     1.0, None, ALU.add, reads=["modc"], writes=["modc"])
            S.barrier()

    def make_hT(self, hT, hk, t0, ntok, sh_j, sc_j, src, src_key):
        nc, S = self.nc, self.S
        m = 1 if t0 < NCTX else 0
        for tb in range(0, ntok, 512):
            nb = min(512, ntok - tb)
            xts = []
            for i in range(nb // 128):
                xt, k = self.xload.get()
                r0 = t0 + tb + i * 128
                S.dma("sp", xt[:], src[r0:r0 + 128, :], reads=[src_key], writes=[k])
                xts.append((xt, k))
            for kc in range(KC):
                ps, pk = self.psB.get()
                for i, (xt, k) in enumerate(xts):
                    S.op("pe", nc.tensor.transpose, ps[:, i * 128:(i + 1) * 128], xt[:, kc * 128:(kc + 1) * 128],
                         self.ident[:], reads=[k, "ident"], writes=[pk])
                S.op("act", nc.scalar.activation, hT[:, kc, tb:tb + nb], ps[:, :nb], AF.Identity,
                     bias=self.modc[:, sh_j + kc, m:m + 1], scale=self.modc[:, sc_j + kc, m:m + 1],
                     reads=[pk, "modc"], writes=[hk])

    def stage_inproj(self, l):
        nc, S = self.nc, self.S
        with ExitStack() as st:
            hp = Pool(nc, st, "hT", [128, KC, 1024], BF16, 1)
            self.xload = Pool(nc, st, "xload", [128, D], F32, 4)
            wp = Pool(nc, st, "win", [128, KC, 512], BF16, 2)
            ep = Pool(nc, st, "pev", [128, 512], F32, 4)
            groups = [(0, NCTX)] + [(NCTX + g * 1024, 1024) for g in range(4)]
            ncb = (IN_W + 511) // 512
            ei = 0
            for (t0, ntok) in groups:
                hT, hk = hp.get()
                self.make_hT(hT, hk, t0, ntok, 0, 16, self.xs, "dram_xs")
                for cb in range(ncb):
                    S.maybe_barrier()
                    c0 = cb * 512
                    cw = min(512, IN_W - c0)
                    wt, wk = wp.get()
                    S.dma("pool", wt[:, :, :cw],
                          self.inp["w_in"][l, :, c0:c0 + cw].rearrange("(kc p) n -> p kc n", p=128), writes=[wk])
                    for cc in range(0, cw, 128):
                        cn = min(128, cw - cc)
                        for tb in range(0, ntok, 512):
                            nb = min(512, ntok - tb)
                            ps, pk = self.psA.get()
                            for kc in range(KC):
                                S.op("pe", nc.tensor.matmul, ps[:cn, :nb], wt[:, kc, cc:cc + cn], hT[:, kc, tb:tb + nb],
                                     start=(kc == 0), stop=(kc == KC - 1), reads=[wk, hk], writes=[pk])
                            et, ek = ep.get()
                            eng = "act" if ei % 2 == 0 else "dve"
                            if eng == "act":
                                S.op("act", nc.scalar.copy, et[:cn, :nb], ps[:cn, :nb], reads=[pk], writes=[ek])
                            else:
                                S.op("dve", nc.vector.tensor_copy, et[:cn, :nb], ps[:cn, :nb], reads=[pk], writes=[ek])
                            ei += 1
                            S.dma("sp", self.PT[c0 + cc:c0 + cc + cn, t0 + tb:t0 + tb + nb], et[:cn, :nb],
                                  reads=[ek], writes=["dram_PT"])
            S.barrier()


    def stage_qkprep(self, l):
        nc, S = self.nc, self.S
        with ExitStack() as st:
            cosT = st.enter_context(nc.sbuf_tensor(uq("sb_cosT"), [128, SEQ], F32))
            sinT = st.enter_context(nc.sbuf_tensor(uq("sb_sinT"), [128, SEQ], F32))
            rotm = st.enter_context(nc.sbuf_tensor(uq("sb_rotm"), [128, 128], F32))
            gq = st.enter_context(nc.sbuf_tensor(uq("sb_gq"), [128, 2], F32))
            S.dma("sp", cosT[:], self.inp["cosT"][:, :], writes=["cosT"])
            S.dma("act", sinT[:], self.inp["sinT"][:, :], writes=["sinT"])
            S.dma("sp", rotm[:], self.inp["rotm"][:, :], writes=["rotm"])
            S.dma("sp", gq[:, 0:1], self.inp["q_norm_a"][l, :].rearrange("(p o) -> p o", o=1), writes=["gq"])
            S.dma("sp", gq[:, 1:2], self.inp["k_norm_a"][l, :].rearrange("(p o) -> p o", o=1), writes=["gq"])
            S.op("dve", nc.vector.tensor_scalar, gq[:, 0:1], gq[:, 0:1], HD ** -0.5, None, ALU.mult,
                 reads=["gq"], writes=["gq"])
            xp = Pool(nc, st, "qx", [128, 512], F32, 3)
            sqp = Pool(nc, st, "qsq", [128, 512], F32, 2)
            rp = Pool(nc, st, "qr", [128, 512], F32, 2)
            xnp = Pool(nc, st, "qxn", [128, 512], F32, 2)
            t1p = Pool(nc, st, "qt1", [128, 512], F32, 2)
            t2p = Pool(nc, st, "qt2", [128, 512], F32, 2)
            obp = Pool(nc, st, "qob", [128, 512], BF16, 3)
            sc = HD ** -0.5
            heads = [(O_AQ + h * 128, True, 0, 1.0) for h in range(8)]
            heads += [(O_AK + g * 128, True, 1, 1.0) for g in range(2)]
            heads += [(O_BQ + h * 128, False, 0, sc) for h in range(8)]
            heads += [(O_BK + g * 128, False, 0, 1.0) for g in range(2)]
            blocks = [(0, NCTX)] + [(NCTX + i * 512, 512) for i in range(8)]
            for hi, (row0, rms, gcol, scl) in enumerate(heads):
                S.maybe_barrier()
                for (t0, nb) in blocks:
                    x, xk = xp.get()
                    S.dma("sp", x[:, :nb], self.PT[row0:row0 + 128, t0:t0 + nb], reads=["dram_PT"], writes=[xk])
                    if rms:
                        sq, sqk = sqp.get()
                        S.op("act", nc.scalar.activation, sq[:, :nb], x[:, :nb], AF.Square, reads=[xk], writes=[sqk])
                        ps, pk = self.psA.get()
                        S.op("pe", nc.tensor.matmul, ps[:, :nb], self.ones[:], sq[:, :nb], start=True, stop=True,
                             reads=["ones", sqk], writes=[pk])
                        r, rk = rp.get()
                        S.op("act", nc.scalar.activation, r[:, :nb], ps[:, :nb], AF.Sqrt, bias=RMS_EPS, scale=1.0 / HD,
                             reads=[pk], writes=[rk])
                        S.op("dve", nc.vector.reciprocal, r[:, :nb], r[:, :nb], reads=[rk], writes=[rk])
                        xn, xnk = xnp.get()
                        S.op("dve", nc.vector.scalar_tensor_tensor, xn[:, :nb], x[:, :nb], gq[:, gcol:gcol + 1], r[:, :nb],
                             ALU.mult, ALU.mult, reads=[xk, "gq", rk], writes=[xnk])
                    else:
                        xn, xnk = x, xk
                    ob, obk = obp.get()
                    if t0 >= NCTX:
                        p0 = t0 - NCTX
                        ps2, pk2 = self.psA.get()
                        S.op("pe", nc.tensor.matmul, ps2[:, :nb], rotm[:], xn[:, :nb], start=True, stop=True,
                             reads=["rotm", xnk], writes=[pk2])
                        t1, t1k = t1p.get()
                        S.op("dve", nc.vector.scalar_tensor_tensor, t1[:, :nb], xn[:, :nb], scl, cosT[:, p0:p0 + nb],
                             ALU.mult, ALU.mult, reads=[xnk, "cosT"], writes=[t1k])
                        t2, t2k = t2p.get()
                        S.op("dve", nc.vector.scalar_tensor_tensor, t2[:, :nb], ps2[:, :nb], scl, sinT[:, p0:p0 + nb],
                             ALU.mult, ALU.mult, reads=[pk2, "sinT"], writes=[t2k])
                        S.op("dve", nc.vector.tensor_tensor, ob[:, :nb], t1[:, :nb], t2[:, :nb], ALU.add,
                             reads=[t1k, t2k], writes=[obk])
                    else:
                        S.op("act", nc.scalar.mul, ob[:, :nb], xn[:, :nb], scl, reads=[xnk], writes=[obk])
                    S.dma("act", self.QK[hi, :, t0:t0 + nb], ob[:, :nb], reads=[obk], writes=["dram_QK"])
            S.barrier()

    def stage_attn(self, l, which, need_ctx):
        nc, S = self.nc, self.S
        isB = which == "B"
        base = 10 if isB else 0
        vrow = O_BV if isB else O_AV
        orow = 1024 if isB else 0
        with ExitStack() as st:
            kT = st.enter_context(nc.sbuf_tensor(uq("sb_kT"), [128, T], BF16))
            vsb = st.enter_context(nc.sbuf_tensor(uq("sb_vsb"), [128, NT, 128], BF16))
            vlp = Pool(nc, st, "vl", [128, 512], F32, 2)
            qp = Pool(nc, st, "aq", [128, 512], BF16, 2)
            pp = Pool(nc, st, "ap", [128, 512], BF16, 4)
            rdp = Pool(nc, st, "ard", [128, 512], F32, 2)
            otp = Pool(nc, st, "aot", [128, 512], BF16, 2)
            if isB:
                wmask = st.enter_context(nc.sbuf_tensor(uq("sb_wmask"), [128, 6, 512], BF16))
                esink = st.enter_context(nc.sbuf_tensor(uq("sb_esink"), [128, 8], F32))
                S.dma("pool", wmask[:], self.inp["wmask"][:, :, :], writes=["wmask"])
                S.dma("sp", esink[:], self.inp["sink_b"][l, :].partition_broadcast(128), writes=["esink"])
                S.op("act", nc.scalar.activation, esink[:], esink[:], AF.Exp, reads=["esink"], writes=["esink"])
            vblocks = [(0, NCTX)] + [(NCTX + i * 512, 512) for i in range(8)]
            for g in range(2):
                S.dma("sp", kT[:], self.QK[base + 8 + g, :, :], reads=["dram_QK"], writes=["kT"])
                for (t0, nb) in vblocks:
                    vl, vk = vlp.get()
                    S.dma("sp", vl[:, :nb], self.PT[vrow + g * 128:vrow + (g + 1) * 128, t0:t0 + nb],
                          reads=["dram_PT"], writes=[vk])
                    ps, pk = self.psA.get()
                    for j in range(nb // 128):
                        S.op("pe", nc.tensor.transpose, ps[:, j * 128:(j + 1) * 128], vl[:, j * 128:(j + 1) * 128],
                             self.ident[:], reads=[vk, "ident"], writes=[pk])
                    tt0 = t0 // 128
                    S.op("act", nc.scalar.copy, vsb[:, tt0:tt0 + nb // 128, :],
                         ps[:, :nb].rearrange("p (j d) -> p j d", d=128), reads=[pk], writes=["vsb"])
                for hh in range(4):
                    S.maybe_barrier()
                    h = g * 4 + hh
                    qblocks = ([(0, NCTX, True)] if need_ctx else []) + [(NCTX + i * 512, 512, False) for i in range(8)]
                    for (t0, nq, isctx) in qblocks:
                        q, qk = qp.get()
                        S.dma("sp", q[:, :nq], self.QK[base + h, :, t0:t0 + nq], reads=["dram_QK"], writes=[qk])
                        if isctx:
                            tiles = [(0, None), (1, None)]
                        elif not isB:
                            tiles = [(kt, None) for kt in range(NT)]
                        else:
                            ql0 = (t0 - NCTX) // 128
                            tiles = [(0, None), (1, None)]
                            for kl in range(ql0 - 1, ql0 + 5):
                                if 0 <= kl < SEQ // 128:
                                    tiles.append((2 + kl, kl - ql0 + 1))
                        po, pok = self.psB.get()
                        pd, pdk = self.psB.get()
                        for ti, (kt, mk) in enumerate(tiles):
                            ps, pk = self.psA.get()
                            S.op("pe", nc.tensor.matmul, ps[:, :nq], kT[:, kt * 128:(kt + 1) * 128], q[:, :nq],
                                 start=True, stop=True, reads=["kT", qk], writes=[pk])
                            p, ppk = pp.get()
                            S.op("act", nc.scalar.activation, p[:, :nq], ps[:, :nq], AF.Exp, reads=[pk], writes=[ppk])
                            if mk is not None and mk != 1 + 0 and False:
                                pass
                            if mk is not None:
                                S.op("pool", nc.gpsimd.tensor_tensor, p[:, :nq], p[:, :nq], wmask[:, mk, :nq], ALU.mult,
                                     reads=[ppk, "wmask"], writes=[ppk])
                            first, last = ti == 0, ti == len(tiles) - 1
                            S.op("pe", nc.tensor.matmul, po[:, :nq], vsb[:, kt, :], p[:, :nq], start=first, stop=last,
                                 reads=["vsb", ppk], writes=[pok])
                            S.op("pe", nc.tensor.matmul, pd[:, :nq], self.onesb[:], p[:, :nq], start=first, stop=last,
                                 reads=["onesb", ppk], writes=[pdk])
                        rd, rdk = rdp.get()
                        if isB:
                            S.op("dve", nc.vector.tensor_scalar, rd[:, :nq], pd[:, :nq], esink[:, h:h + 1], None, ALU.add,
                                 reads=[pdk, "esink"], writes=[rdk])
                            S.op("dve", nc.vector.reciprocal, rd[:, :nq], rd[:, :nq], reads=[rdk], writes=[rdk])
                        else:
                            S.op("dve", nc.vector.reciprocal, rd[:, :nq], pd[:, :nq], reads=[pdk], writes=[rdk])
                        ot, otk = otp.get()
                        S.op("dve", nc.vector.tensor_tensor, ot[:, :nq], po[:, :nq], rd[:, :nq], ALU.mult,
                             reads=[pok, rdk], writes=[otk])
                        S.dma("act", self.OT[orow + h * 128:orow + (h + 1) * 128, t0:t0 + nq], ot[:, :nq],
                              reads=[otk], writes=["dram_OT"])
            S.barrier()


    def stage_delta_prep(self, l):
        nc, S = self.nc, self.S
        with ExitStack() as st:
            wc = st.enter_context(nc.sbuf_tensor(uq("sb_wc"), [128, 16, 5], F32))
            for j in range(5):
                S.dma("sp", wc[:, :, j], self.inp["conv_c"][l, j, :].rearrange("(c p) -> p c", p=128), writes=["wc"],
                      allow_slow_non_contiguous=True)
            xpp = Pool(nc, st, "cxp", [128, T + 8], F32, 2)
            acp = Pool(nc, st, "cac", [128, T], F32, 2)
            sqp = Pool(nc, st, "csq", [128, 512], F32, 2)
            rp = Pool(nc, st, "cr", [128, 512], F32, 2)
            tmp = Pool(nc, st, "ctm", [128, 4, 128], F32, 3)
            for (xp_, k) in xpp.bufs:
                S.op("pool", nc.gpsimd.memset, xp_[:], 0.0, writes=[k])
            blocks = [(0, NCTX)] + [(NCTX + i * 512, 512) for i in range(8)]
            for fc in range(16):
                row0 = O_CQ + fc * 128
                xp_, xk = xpp.get()
                S.dma("sp", xp_[:, 2:2 + NCTX], self.PT[row0:row0 + 128, 0:NCTX], reads=["dram_PT"], writes=[xk])
                S.dma("act", xp_[:, 262:262 + SEQ], self.PT[row0:row0 + 128, NCTX:T], reads=["dram_PT"], writes=[xk])
                ac, ak = acp.get()
                for (o0, n, s0) in ((0, NCTX, 0), (NCTX, SEQ, 260)):
                    S.op("dve", nc.vector.tensor_scalar, ac[:, o0:o0 + n], xp_[:, s0:s0 + n], wc[:, fc, 0:1], None, ALU.mult,
                         reads=[xk, "wc"], writes=[ak])
                    for j in range(1, 5):
                        S.op("dve", nc.vector.scalar_tensor_tensor, ac[:, o0:o0 + n], xp_[:, s0 + j:s0 + j + n],
                             wc[:, fc, j:j + 1], ac[:, o0:o0 + n], ALU.mult, ALU.add, reads=[xk, "wc", ak], writes=[ak])
                S.op("act", nc.scalar.activation, ac[:, :], ac[:, :], AF.Silu, reads=[ak], writes=[ak])
                if fc < 8:
                    scl = HD ** -0.5 if fc < 4 else 1.0
                    for (t0, nb) in blocks:
                        sq, sqk = sqp.get()
                        S.op("act", nc.scalar.activation, sq[:, :nb], ac[:, t0:t0 + nb], AF.Square, reads=[ak], writes=[sqk])
                        ps, pk = self.psA.get()
                        S.op("pe", nc.tensor.matmul, ps[:, :nb], self.ones[:], sq[:, :nb], start=True, stop=True,
                             reads=["ones", sqk], writes=[pk])
                        r, rk = rp.get()
                        S.op("act", nc.scalar.activation, r[:, :nb], ps[:, :nb], AF.Sqrt, bias=RMS_EPS, scale=1.0,
                             reads=[pk], writes=[rk])
                        S.op("dve", nc.vector.reciprocal, r[:, :nb], r[:, :nb], reads=[rk], writes=[rk])
                        S.op("dve", nc.vector.scalar_tensor_tensor, ac[:, t0:t0 + nb], ac[:, t0:t0 + nb], scl, r[:, :nb],
                             ALU.mult, ALU.mult, reads=[ak, rk], writes=[ak])
                    dst = self.CQT if fc < 4 else self.CKT
                    S.dma("sp", dst[fc % 4, :, :], ac[:, :], reads=[ak], writes=["dram_CQKT"])
                if fc >= 4:
                    dst = self.CKtm[fc - 4] if fc < 8 else self.CVtm[fc - 8]
                    for (t0, nb) in blocks:
                        ps, pk = self.psA.get()
                        nj = nb // 128
                        for j in range(nj):
                            S.op("pe", nc.tensor.transpose, ps[:, j * 128:(j + 1) * 128],
                                 ac[:, t0 + j * 128:t0 + (j + 1) * 128], self.ident[:], reads=[ak, "ident"], writes=[pk])
                        tm, tk = tmp.get()
                        S.op("act", nc.scalar.copy, tm[:, :nj, :], ps[:, :nb].rearrange("p (j d) -> p j d", d=128),
                             reads=[pk], writes=[tk])
                        S.dma("sp", dst[t0:t0 + nb, :].rearrange("(j t) d -> t j d", t=128), tm[:, :nj, :],
                              reads=[tk], writes=["dram_Ctm"])
            S.barrier()

    def stage_delta_scan(self, l, need_ctx):
        nc, S = self.nc, self.S
        NCH = NT
        with ExitStack() as st:
            def sb(name, shape, dt=F32):
                return st.enter_context(nc.sbuf_tensor(uq(name), shape, dt))
            tri = [sb("sb_trif", [128, 128]), sb("sb_trib", [128, 128])]
            negm = [sb("sb_negf", [128, 128]), sb("sb_negb", [128, 128])]
            offd = sb("sb_offd", [128, 128])
            for t_, nm in ((tri[0], "tri_f"), (tri[1], "tri_b"), (negm[0], "negm_f"), (negm[1], "negm_b"), (offd, "offdiag")):
                S.dma("sp", t_[:], self.inp[nm][:, :], writes=["dconst"])
            ab = sb("sb_ab", [32, T])
            S.dma("sp", ab[:], self.PT[O_CA:O_CA + 32, :], reads=["dram_PT"], writes=["ab"])
            abtm = sb("sb_abtm", [128, NCH, 32])
            G = sb("sb_G", [128, NCH, 16])
            Bt = sb("sb_Bt", [128, NCH, 16])
            GC = sb("sb_GC", [128, NCH, 16])
            TOT = sb("sb_TOT", [128, NCH, 16])
            EG = sb("sb_EG", [128, NCH, 16])
            BG = sb("sb_BG", [128, NCH, 16])
            EKD = sb("sb_EKD", [128, NCH, 16])
            ETOT = sb("sb_ETOT", [128, NCH, 16])
            dtb = sb("sb_dtb", [128, 16])
            nal = sb("sb_nal", [128, 16])
            S.dma("sp", dtb[:], self.inp["dt_bias_c"][l].rearrange("a b -> (a b)").partition_broadcast(128), writes=["dtb"])
            S.dma("sp", nal[:], self.inp["a_log_c"][l].rearrange("a b -> (a b)").partition_broadcast(128), writes=["nal"])
            S.op("act", nc.scalar.activation, nal[:], nal[:], AF.Exp, reads=["nal"], writes=["nal"])
            S.op("dve", nc.vector.tensor_scalar, nal[:], nal[:], -1.0, None, ALU.mult, reads=["nal"], writes=["nal"])
            for c0 in range(0, NCH, 16):
                n = min(16, NCH - c0)
                ps, pk = self.psA.get()
                for c in range(n):
                    S.op("pe", nc.tensor.transpose, ps[:, c * 32:(c + 1) * 32], ab[0:32, (c0 + c) * 128:(c0 + c + 1) * 128],
                         self.ident[0:32, 0:32], reads=["ab", "ident"], writes=[pk])
                S.op("act", nc.scalar.copy, abtm[:, c0:c0 + n, :], ps[:, :n * 32].rearrange("p (c k) -> p c k", k=32),
                     reads=[pk], writes=["abtm"])
            for c in range(NCH):
                S.op("dve", nc.vector.tensor_tensor, G[:, c, :], abtm[:, c, 0:16], dtb[:], ALU.add,
                     reads=["abtm", "dtb"], writes=["G"])
            S.op("act", nc.scalar.activation, G[:], G[:], AF.Exp, reads=["G"], writes=["G"])
            S.op("act", nc.scalar.activation, G[:], G[:], AF.Ln, bias=1.0, reads=["G"], writes=["G"])
            for c in range(NCH):
                S.op("dve", nc.vector.tensor_tensor, G[:, c, :], G[:, c, :], nal[:], ALU.mult,
                     reads=["G", "nal"], writes=["G"])
            S.op("act", nc.scalar.activation, Bt[:], abtm[:, :, 16:32], AF.Sigmoid, reads=["abtm"], writes=["Bt"])
            for c0 in range(0, NCH, 32):
                n = min(32, NCH - c0)
                ps, pk = self.psA.get()
                ps2, pk2 = self.psA.get()
                for c in range(n):
                    for d in range(2):
                        S.op("pe", nc.tensor.matmul, ps[:, c * 16 + d * 8:c * 16 + d * 8 + 8], tri[d][:],
                             G[:, c0 + c, d * 8:d * 8 + 8], start=True, stop=True, reads=["dconst", "G"], writes=[pk])
                    S.op("pe", nc.tensor.matmul, ps2[:, c * 16:(c + 1) * 16], self.ones[:], G[:, c0 + c, :],
                         start=True, stop=True, reads=["ones", "G"], writes=[pk2])
                S.op("act", nc.scalar.copy, GC[:, c0:c0 + n, :], ps[:, :n * 16].rearrange("p (c k) -> p c k", k=16),
                     reads=[pk], writes=["GC"])
                S.op("act", nc.scalar.copy, TOT[:, c0:c0 + n, :], ps2[:, :n * 16].rearrange("p (c k) -> p c k", k=16),
                     reads=[pk2], writes=["TOT"])
            S.op("act", nc.scalar.activation, EG[:], GC[:], AF.Exp, reads=["GC"], writes=["EG"])
            S.op("dve", nc.vector.tensor_tensor, BG[:], Bt[:], EG[:], ALU.mult, reads=["Bt", "EG"], writes=["BG"])
            S.op("dve", nc.vector.tensor_tensor, EKD[:], TOT[:], GC[:], ALU.subtract, reads=["TOT", "GC"], writes=["EKD"])
            S.op("act", nc.scalar.activation, EKD[:], EKD[:], AF.Exp, reads=["EKD"], writes=["EKD"])
            S.op("act", nc.scalar.activation, ETOT[:], TOT[:], AF.Exp, reads=["TOT"], writes=["ETOT"])
            gk = ["GC", "Bt", "EG", "BG", "EKD", "ETOT"]
            if "GDBG" in self.dbg:
                gd = self.dram("GDBG", [6, 128, NCH * 16])
                for i_, t_ in enumerate((GC, Bt, EG, BG, EKD, ETOT)):
                    S.dma("sp", gd[i_], t_[:].rearrange("p c k -> p (c k)"), reads=gk, writes=["dram_GDBG"])
            if os.environ.get("MK_SCAN", "full") == "gates":
                S.barrier()
                return
            nchunks_dbg = int(os.environ.get("MK_SCAN_N", "1000"))
            bi = [0]
            banks = self.psA.bufs + self.psB.bufs

            def BANK():
                r = banks[bi[0] % len(banks)]
                bi[0] += 1
                return r
            PC = {}
            for d in range(2):
                for g in range(2):
                    pc = {r: Pool(nc, st, "d%d%d_%s" % (d, g, r), [128, 4, 128], F32, 1) for r in ("A", "B", "C", "D", "E")}
                    for r in ("X", "XT", "TT"):
                        pc[r] = Pool(nc, st, "d%d%d_%s" % (d, g, r), [128, 4, 128], F32, 2)
                    PC[(d, g)] = pc
            LD = {}
            for d in range(2):
                LD[d] = {"qT": Pool(nc, st, "d%d_qT" % d, [128, 4, 128], F32, 1),
                         "kT": Pool(nc, st, "d%d_kT" % d, [128, 4, 128], F32, 1),
                         "ktm": Pool(nc, st, "d%d_ktm" % d, [128, 4, 128], F32, 1),
                         "vtm": Pool(nc, st, "d%d_vtm" % d, [128, 8, 128], F32, 1),
                         "KK": Pool(nc, st, "d%d_KK" % d, [128, 8, 128], F32, 1),
                         "QKT": Pool(nc, st, "d%d_QKT" % d, [128, 8, 128], F32, 1),
                         "ob": Pool(nc, st, "d%d_ob" % d, [128, 8, 128], F32, 1)}
            Sst = sb("sb_state", [128, 16, 128])
            ident4 = sb("sb_ident4", [128, 4, 128])
            negm4 = [sb("sb_negm4f", [128, 4, 128]), sb("sb_negm4b", [128, 4, 128])]
            for i in range(4):
                S.op("pool", nc.gpsimd.tensor_copy, ident4[:, i, :], self.ident[:], reads=["ident"], writes=["dconst4"])
                for d in range(2):
                    S.op("pool", nc.gpsimd.tensor_copy, negm4[d][:, i, :], negm[d][:], reads=["dconst"], writes=["dconst4"])
            ident, ones = self.ident, self.ones

            def flat(t):
                return t[:].rearrange("p h d -> p (h d)")

            def chain(d, c, g, L):
                qT, qTk = L["qT"]
                ktm, ktmk = L["ktm"]
                vtm, vtmk = L["vtm"]
                KK, KKk = L["KK"]
                QKT, QKTk = L["QKT"]
                ob, obk = L["ob"]
                pc = PC[(d, g)]
                hvs = [g * 4 + i for i in range(4)]
                cols = [d * 8 + hv for hv in hvs]
                sidx = [d * 8 + hv for hv in hvs]
                sk = "S%d%d" % (d, g)
                dg, dgk = pc["A"].get()
                for i in range(4):
                    S.op("act", nc.scalar.activation, dg[:, i, :], ident[:], AF.Identity, scale=GC[:, c, cols[i]:cols[i] + 1],
                         reads=["ident"] + gk, writes=[dgk])
                b1, b1k = BANK()
                for i in range(4):
                    S.op("pe", nc.tensor.matmul, b1[:, i * 128:(i + 1) * 128], ones[:], dg[:, i, :], start=True, stop=True,
                         reads=["ones", dgk], writes=[b1k])
                yield
                tt_, ttk = pc["B"].get()
                S.op("dve", nc.vector.scalar_tensor_tensor, flat(tt_), b1[:, :], -1.0, flat(negm4[d]), ALU.mult, ALU.add,
                     reads=[b1k, "dconst4"], writes=[ttk])
                Di, Dik = pc["C"].get()
                for i in range(4):
                    S.op("act", nc.scalar.activation, Di[:, i, :], tt_[:, i, :], AF.Exp, bias=GC[:, c, cols[i]:cols[i] + 1],
                         reads=[ttk] + gk, writes=[Dik])
                Dm, Dmk = pc["A"].get()
                for i in range(4):
                    S.op("dve", nc.vector.scalar_tensor_tensor, Dm[:, i, :], Di[:, i, :], Bt[:, c, cols[i]:cols[i] + 1], offd[:],
                         ALU.mult, ALU.mult, reads=[Dik, "dconst"] + gk, writes=[Dmk])
                X, Xk = pc["X"].get()
                S.op("dve", nc.vector.scalar_tensor_tensor, flat(X), KK[:, g * 4:g * 4 + 4, :].rearrange("p h d -> p (h d)"), -1.0,
                     flat(Dm), ALU.mult, ALU.mult, reads=[KKk, Dmk], writes=[Xk])
                b2, b2k = BANK()
                for i in range(4):
                    S.op("pe", nc.tensor.transpose, b2[:, i * 128:(i + 1) * 128], X[:, i, :], ident[:], reads=[Xk, "ident"], writes=[b2k])
                b3, b3k = BANK()
                for i in range(4):
                    S.op("pe", nc.tensor.transpose, b3[:, i * 128:(i + 1) * 128], Di[:, i, :], ident[:], reads=[Dik, "ident"], writes=[b3k])
                yield
                XT, XTk = pc["XT"].get()
                S.op("act", nc.scalar.copy, flat(XT), b2[:, :], reads=[b2k], writes=[XTk])
                TT, TTk = pc["TT"].get()
                S.op("dve", nc.vector.tensor_tensor, flat(TT), flat(XT), flat(ident4), ALU.add, reads=[XTk, "dconst4"], writes=[TTk])
                QKd, QKdk = pc["D"].get()
                S.op("dve", nc.vector.tensor_tensor, flat(QKd), b3[:, :], QKT[:, g * 4:g * 4 + 4, :].rearrange("p h d -> p (h d)"),
                     ALU.mult, reads=[b3k, QKTk], writes=[QKdk])
                for lev in range(1, 7):
                    b4, b4k = BANK()
                    for i in range(4):
                        S.op("pe", nc.tensor.matmul, b4[:, i * 128:(i + 1) * 128], XT[:, i, :], X[:, i, :], start=True, stop=True,
                             reads=[XTk, Xk], writes=[b4k])
                    if lev < 6:
                        b5, b5k = BANK()
                        for i in range(4):
                            S.op("pe", nc.tensor.matmul, b5[:, i * 128:(i + 1) * 128], X[:, i, :], XT[:, i, :], start=True, stop=True,
                                 reads=[XTk, Xk], writes=[b5k])
                    yield
                    Xn, Xnk = pc["X"].get()
                    S.op("act", nc.scalar.copy, flat(Xn), b4[:, :], reads=[b4k], writes=[Xnk])
                    if lev < 6:
                        XTn, XTnk = pc["XT"].get()
                        S.op("dve", nc.vector.tensor_copy, flat(XTn), b5[:, :], reads=[b5k], writes=[XTnk])
                    b6, b6k = BANK()
                    for i in range(4):
                        S.op("pe", nc.tensor.matmul, b6[:, i * 128:(i + 1) * 128], Xn[:, i, :], TT[:, i, :], start=True, stop=True,
                             reads=[Xnk, TTk], writes=[b6k])
                    yield
                    TTn, TTnk = pc["TT"].get()
                    S.op("dve", nc.vector.tensor_tensor, flat(TTn), b6[:, :], flat(TT), ALU.add, reads=[b6k, TTk], writes=[TTnk])
                    X, Xk = Xn, Xnk
                    if lev < 6:
                        XT, XTk = XTn, XTnk
                    TT, TTk = TTn, TTnk
                vb, vbk = pc["B"].get()
                kbg, kbgk = pc["C"].get()
                kdec, kdeck = pc["E"].get()
                for i in range(4):
                    hv = hvs[i]
                    hq = hv // 2
                    S.op("pool", nc.gpsimd.tensor_scalar, vb[:, i, :], vtm[:, hv, :], Bt[:, c, cols[i]:cols[i] + 1], None, ALU.mult,
                         reads=[vtmk] + gk, writes=[vbk])
                    S.op("pool", nc.gpsimd.tensor_scalar, kbg[:, i, :], ktm[:, hq, :], BG[:, c, cols[i]:cols[i] + 1], None, ALU.mult,
                         reads=[ktmk] + gk, writes=[kbgk])
                    S.op("pool", nc.gpsimd.tensor_scalar, kdec[:, i, :], ktm[:, hq, :], EKD[:, c, cols[i]:cols[i] + 1], None, ALU.mult,
                         reads=[ktmk] + gk, writes=[kdeck])
                b7, b7k = BANK()
                for i in range(4):
                    S.op("pe", nc.tensor.matmul, b7[:, i * 128:(i + 1) * 128], TT[:, i, :], vb[:, i, :], start=True, stop=True,
                         reads=[TTk, vbk], writes=[b7k])
                b8, b8k = BANK()
                for i in range(4):
                    S.op("pe", nc.tensor.matmul, b8[:, i * 128:(i + 1) * 128], kbg[:, i, :], TT[:, i, :], start=True, stop=True,
                         reads=[TTk, kbgk], writes=[b8k])
                yield
                u, uk = pc["A"].get()
                S.op("act", nc.scalar.copy, flat(u), b7[:, :], reads=[b7k], writes=[uk])
                wT, wTk = pc["B"].get()
                S.op("dve", nc.vector.tensor_copy, flat(wT), b8[:, :], reads=[b8k], writes=[wTk])
                b9, b9k = BANK()
                for i in range(4):
                    S.op("pe", nc.tensor.matmul, b9[:, i * 128:(i + 1) * 128], wT[:, i, :], Sst[:, sidx[i], :], start=True, stop=True,
                         reads=[wTk, sk], writes=[b9k])
                b10, b10k = BANK()
                for i in range(4):
                    S.op("pe", nc.tensor.matmul, b10[:, i * 128:(i + 1) * 128], qT[:, hvs[i] // 2, :], Sst[:, sidx[i], :], start=True, stop=True,
                         reads=[qTk, sk], writes=[b10k])
                yield
                vn, vnk = pc["C"].get()
                S.op("dve", nc.vector.tensor_tensor, flat(vn), flat(u), b9[:, :], ALU.subtract, reads=[uk, b9k], writes=[vnk])
                o1s, o1sk = pc["A"].get()
                for i in range(4):
                    S.op("act", nc.scalar.activation, o1s[:, i, :], b10[:, i * 128:(i + 1) * 128], AF.Identity,
                         scale=EG[:, c, cols[i]:cols[i] + 1], reads=[b10k] + gk, writes=[o1sk])
                b11, b11k = BANK()
                for i in range(4):
                    S.op("pe", nc.tensor.matmul, b11[:, i * 128:(i + 1) * 128], QKd[:, i, :], vn[:, i, :], start=True, stop=True,
                         reads=[QKdk, vnk], writes=[b11k])
                b12, b12k = BANK()
                for i in range(4):
                    S.op("pe", nc.tensor.matmul, b12[:, i * 128:(i + 1) * 128], kdec[:, i, :], vn[:, i, :], start=True, stop=True,
                         reads=[kdeck, vnk], writes=[b12k])
                yield
                S.op("dve", nc.vector.tensor_tensor, ob[:, g * 4:g * 4 + 4, :].rearrange("p h d -> p (h d)"), b11[:, :], flat(o1s), ALU.add,
                     reads=[b11k, o1sk], writes=[obk + "_g%d" % g])
                for i in range(4):
                    S.op("dve", nc.vector.scalar_tensor_tensor, Sst[:, sidx[i], :], Sst[:, sidx[i], :], ETOT[:, c, cols[i]:cols[i] + 1],
                         b12[:, i * 128:(i + 1) * 128], ALU.mult, ALU.add, reads=[sk, b12k] + gk, writes=[sk])

            S.op("pool", nc.gpsimd.memset, Sst[:], 0.0, writes=["S00", "S01", "S10", "S11"])
            orders = [[0, 1] + list(range(2, NCH)), [1, 0] + list(range(NCH - 1, 1, -1))]
            for t in range(min(NCH, nchunks_dbg)):
                gens = []
                Ls = []
                for d in range(2):
                    c = orders[d][t]
                    t0 = c * 128
                    L = {k_: LD[d][k_].get() for k_ in LD[d]}
                    qT, qTk = L["qT"]
                    kT, kTk = L["kT"]
                    ktm, ktmk = L["ktm"]
                    vtm, vtmk = L["vtm"]
                    KK, KKk = L["KK"]
                    QKT, QKTk = L["QKT"]
                    obk = L["ob"][1]
                    S.dma("sp", qT[:], self.CQT[:, :, t0:t0 + 128].rearrange("h d t -> d h t"), reads=["dram_CQKT"], writes=[qTk])
                    S.dma("sp", kT[:], self.CKT[:, :, t0:t0 + 128].rearrange("h d t -> d h t"), reads=["dram_CQKT"], writes=[kTk])
                    S.dma("act", ktm[:], self.CKtm[:, t0:t0 + 128, :].rearrange("h t d -> t h d"), reads=["dram_Ctm"], writes=[ktmk])
                    S.dma("act", vtm[:], self.CVtm[:, t0:t0 + 128, :].rearrange("h t d -> t h d"), reads=["dram_Ctm"], writes=[vtmk])
                    bk1, bk1k = BANK()
                    for hq in range(4):
                        S.op("pe", nc.tensor.matmul, bk1[:, hq * 128:(hq + 1) * 128], kT[:, hq, :], kT[:, hq, :],
                             start=True, stop=True, reads=[kTk], writes=[bk1k])
                    bk2, bk2k = BANK()
                    for hq in range(4):
                        S.op("pe", nc.tensor.matmul, bk2[:, hq * 128:(hq + 1) * 128], kT[:, hq, :], qT[:, hq, :],
                             start=True, stop=True, reads=[kTk, qTk], writes=[bk2k])
                    for rep in range(2):
                        S.op("act", nc.scalar.copy, KK[:].rearrange("p (h r) d -> p h r d", r=2)[:, :, rep, :],
                             bk1[:, :].rearrange("p (h d) -> p h d", d=128), reads=[bk1k], writes=[KKk])
                        S.op("dve", nc.vector.tensor_copy, QKT[:].rearrange("p (h r) d -> p h r d", r=2)[:, :, rep, :],
                             bk2[:, :].rearrange("p (h d) -> p h d", d=128), reads=[bk2k], writes=[QKTk])
                    S._deps("dve", [], [obk])
                    Ls.append((d, c, L))
                    for g in range(2):
                        gens.append(chain(d, c, g, L))
                alive = list(gens)
                while alive:
                    nxt = []
                    for gen in alive:
                        try:
                            next(gen)
                            nxt.append(gen)
                        except StopIteration:
                            pass
                    alive = nxt
                for (d, c, L) in Ls:
                    ob, obk = L["ob"]
                    if c >= 2 or need_ctx:
                        S.dma("sp", self.OC[d][c * 128:(c + 1) * 128, :], ob[:].rearrange("p h d -> p (h d)"),
                              reads=[obk + "_g0", obk + "_g1"], writes=["dram_OC", obk])
            S.barrier()

    def stage_delta_out(self, l, need_ctx):
        nc, S = self.nc, self.S
        with ExitStack() as st:
            ncol = st.enter_context(nc.sbuf_tensor(uq("sb_ncol"), [128, 1], F32))
            S.dma("sp", ncol[:], self.inp["norm_c"][l, :].rearrange("(p o) -> p o", o=1), writes=["ncol"])
            o0p = Pool(nc, st, "go0", [128, 4, 128], F32, 2)
            o1p = Pool(nc, st, "go1", [128, 4, 128], F32, 2)
            xp = Pool(nc, st, "gx", [128, 512], F32, 2)
            sqp = Pool(nc, st, "gsq", [128, 512], F32, 2)
            rp = Pool(nc, st, "gr", [128, 512], F32, 2)
            zp = Pool(nc, st, "gz", [128, 512], F32, 2)
            obp = Pool(nc, st, "gob", [128, 512], BF16, 2)
            blocks = ([(0, NCTX)] if need_ctx else []) + [(NCTX + i * 512, 512) for i in range(8)]
            for hv in range(8):
                for (t0, nb) in blocks:
                    nj = nb // 128
                    o0, o0k = o0p.get()
                    o1, o1k = o1p.get()
                    S.dma("sp", o0[:, :nj, :], self.OC[0][t0:t0 + nb, hv * 128:(hv + 1) * 128].rearrange("(j t) d -> t j d", t=128),
                          reads=["dram_OC"], writes=[o0k])
                    S.dma("act", o1[:, :nj, :], self.OC[1][t0:t0 + nb, hv * 128:(hv + 1) * 128].rearrange("(j t) d -> t j d", t=128),
                          reads=["dram_OC"], writes=[o1k])
                    S.op("pool", nc.gpsimd.tensor_tensor, o0[:, :nj, :], o0[:, :nj, :], o1[:, :nj, :], ALU.add,
                         reads=[o0k, o1k], writes=[o0k])
                    ps, pk = self.psA.get()
                    for j in range(nj):
                        S.op("pe", nc.tensor.transpose, ps[:, j * 128:(j + 1) * 128], o0[:, j, :], self.ident[:],
                             reads=[o0k, "ident"], writes=[pk])
                    x, xk = xp.get()
                    S.op("dve", nc.vector.tensor_copy, x[:, :nb], ps[:, :nb], reads=[pk], writes=[xk])
                    sq, sqk = sqp.get()
                    S.op("act", nc.scalar.activation, sq[:, :nb], x[:, :nb], AF.Square, reads=[xk], writes=[sqk])
                    ps2, pk2 = self.psA.get()
                    S.op("pe", nc.tensor.matmul, ps2[:, :nb], self.ones[:], sq[:, :nb], start=True, stop=True,
                         reads=["ones", sqk], writes=[pk2])
                    r, rk = rp.get()
                    S.op("act", nc.scalar.activation, r[:, :nb], ps2[:, :nb], AF.Sqrt, bias=RMS_EPS, scale=1.0 / HD,
                         reads=[pk2], writes=[rk])
                    S.op("dve", nc.vector.reciprocal, r[:, :nb], r[:, :nb], reads=[rk], writes=[rk])
                    z, zk = zp.get()
                    S.dma("sp", z[:, :nb], self.PT[O_CZ + hv * 128:O_CZ + (hv + 1) * 128, t0:t0 + nb], reads=["dram_PT"], writes=[zk])
                    S.op("act", nc.scalar.activation, z[:, :nb], z[:, :nb], AF.Silu, reads=[zk], writes=[zk])
                    S.op("dve", nc.vector.scalar_tensor_tensor, x[:, :nb], x[:, :nb], ncol[:, 0:1], r[:, :nb], ALU.mult, ALU.mult,
                         reads=[xk, "ncol", rk], writes=[xk])
                    ob, obk = obp.get()
                    S.op("dve", nc.vector.tensor_tensor, ob[:, :nb], x[:, :nb], z[:, :nb], ALU.mult, reads=[xk, zk], writes=[obk])
                    S.dma("act", self.OT[2048 + hv * 128:2048 + (hv + 1) * 128, t0:t0 + nb], ob[:, :nb], reads=[obk], writes=["dram_OT"])
            S.barrier()


    def stage_merge(self, l, need_ctx):
        nc, S = self.nc, self.S
        with ExitStack() as st:
            wbr = Pool(nc, st, "wbr", [128, 8, D], BF16, 2)
            otp = Pool(nc, st, "mot", [128, 8, 512], BF16, 2)
            gp = Pool(nc, st, "mg", [128, 512], F32, 4)
            tp = Pool(nc, st, "mt", [128, 512], F32, 3)
            accp = Pool(nc, st, "macc", [128, KC, 512], F32, 1)
            ybp = Pool(nc, st, "myb", [128, KC, 512], BF16, 2)
            blocks = ([(0, NCTX)] if need_ctx else []) + [(NCTX + i * 512, 512) for i in range(8)]
            wnames = ["w_br_a", "w_br_b", "w_br_c"]
            for (t0, nb) in blocks:
                acc, acck = accp.get()
                for br in range(3):
                    wt, wk = wbr.get()
                    S.dma("pool", wt[:], self.inp[wnames[br]][l].rearrange("(kc p) n -> p kc n", p=128), writes=[wk])
                    ot, otk = otp.get()
                    S.dma("sp", ot[:, :, :nb], self.OT[br * 1024:(br + 1) * 1024, t0:t0 + nb].rearrange("(kc p) t -> p kc t", p=128),
                          reads=["dram_OT"], writes=[otk])
                    for oc in range(KC):
                        ps, pk = self.psA.get()
                        for kc in range(8):
                            S.op("pe", nc.tensor.matmul, ps[:, :nb], wt[:, kc, oc * 128:(oc + 1) * 128], ot[:, kc, :nb],
                                 start=(kc == 0), stop=(kc == 7), reads=[wk, otk], writes=[pk])
                        g, gk_ = gp.get()
                        r0 = O_G + br * D + oc * 128
                        S.dma("act" if oc % 2 else "sp", g[:, :nb], self.PT[r0:r0 + 128, t0:t0 + nb], reads=["dram_PT"], writes=[gk_])
                        S.op("act", nc.scalar.activation, g[:, :nb], g[:, :nb], AF.Sigmoid, reads=[gk_], writes=[gk_])
                        if br == 0:
                            S.op("dve", nc.vector.tensor_tensor, acc[:, oc, :nb], ps[:, :nb], g[:, :nb], ALU.mult,
                                 reads=[pk, gk_], writes=[acck])
                        else:
                            t_, tk = tp.get()
                            S.op("dve", nc.vector.tensor_tensor, t_[:, :nb], ps[:, :nb], g[:, :nb], ALU.mult,
                                 reads=[pk, gk_], writes=[tk])
                            S.op("pool", nc.gpsimd.tensor_tensor, acc[:, oc, :nb], acc[:, oc, :nb], t_[:, :nb], ALU.add,
                                 reads=[tk, acck], writes=[acck])
                yb, ybk = ybp.get()
                S.op("act", nc.scalar.copy, yb[:, :, :nb], acc[:, :, :nb], reads=[acck], writes=[ybk])
                S.dma("sp", self.YP[:, t0:t0 + nb].rearrange("(kc p) t -> p kc t", p=128), yb[:, :, :nb],
                      reads=[ybk], writes=["dram_YP"])
            S.barrier()

    def load_bc(self, st, name, src_row):
        nc, S = self.nc, self.S
        t = st.enter_context(nc.sbuf_tensor(uq("sb_bc_" + name), [128, D], F32))
        k = "bc_" + name
        S.dma("sp", t[:], src_row.partition_broadcast(128), reads=["dram_modd"], writes=[k])
        return t, k

    def stage_wout_ln(self, l, need_ctx):
        nc, S = self.nc, self.S
        with ExitStack() as st:
            wo = st.enter_context(nc.sbuf_tensor(uq("sb_wo"), [128, KC, D], BF16))
            for q in range(4):
                S.dma("pool", wo[:, :, q * 512:(q + 1) * 512],
                      self.inp["w_out"][l, :, q * 512:(q + 1) * 512].rearrange("(kc p) n -> p kc n", p=128), writes=["wo"])
            gt = [self.load_bc(st, "gt1l", self.modd[l, 0, 2 * D:3 * D]), self.load_bc(st, "gt1c", self.modd[l, 1, 2 * D:3 * D])]
            lng, lngk = self.load_bc(st, "ln1g", self.inp["ln1_g"][l, :])
            lnb, lnbk = self.load_bc(st, "ln1b", self.inp["ln1_b"][l, :])
            ybp = Pool(nc, st, "wyb", [128, KC, 512], BF16, 2)
            xp = Pool(nc, st, "wx", [128, D], F32, 2)
            zp = Pool(nc, st, "wz", [128, D], F32, 2)
            junk = Pool(nc, st, "wjunk", [128, D], F32, 1)
            blocks = ([(0, NCTX)] if need_ctx else []) + [(NCTX + i * 512, 512) for i in range(8)]
            for (t0, nb) in blocks:
                yb, ybk = ybp.get()
                S.dma("sp", yb[:, :, :nb], self.YP[:, t0:t0 + nb].rearrange("(kc p) t -> p kc t", p=128),
                      reads=["dram_YP"], writes=[ybk])
                gtt, gtk = gt[1] if t0 < NCTX else gt[0]
                for j in range(nb // 128):
                    tok0 = t0 + j * 128
                    xt, xk = xp.get()
                    S.dma("act", xt[:], self.xs[tok0:tok0 + 128, :], reads=["dram_xs"], writes=[xk])
                    zt, zk = zp.get()
                    for q in range(4):
                        ps, pk = self.psA.get()
                        for kc in range(KC):
                            S.op("pe", nc.tensor.matmul, ps[:, :], yb[:, kc, j * 128:(j + 1) * 128], wo[:, kc, q * 512:(q + 1) * 512],
                                 start=(kc == 0), stop=(kc == KC - 1), reads=[ybk, "wo"], writes=[pk])
                        S.op("dve", nc.vector.tensor_tensor, zt[:, q * 512:(q + 1) * 512], ps[:, :], gtt[:, q * 512:(q + 1) * 512],
                             ALU.mult, reads=[pk, gtk], writes=[zk])
                    S.op("dve", nc.vector.scalar_tensor_tensor, zt[:], xt[:], ALPHA, zt[:], ALU.mult, ALU.add,
                         reads=[xk, zk], writes=[zk])
                    self.ln_rows(S, zt, zk, self.small, gb=(lng, lnb, lngk, lnbk), junk=junk)
                    S.dma("sp", self.xs[tok0:tok0 + 128, :], zt[:], reads=[zk], writes=["dram_xs"])
            S.barrier()

    def stage_moe(self, l, need_ctx):
        nc, S = self.nc, self.S
        last = l == DEPTH - 1
        tiles = list(range(0 if need_ctx else 2, NT))
        ntl = len(tiles)
        with ExitStack() as st0:
            def sbp(name, shape, dt=F32):
                return st0.enter_context(nc.sbuf_tensor(uq(name), shape, dt))
            SC = sbp("sb_SC", [128, NT, NE])
            M = sbp("sb_M", [128, NT, NE])
            IDXF = sbp("sb_IDXF", [128, NT, 8])
            DEST = sbp("sb_DEST", [128, NT, 8], I32)
            GATE = sbp("sb_GATE", [128, NT, 8])
            be_i = sbp("sb_bei", [128, NBIG], I32)
            start_bc = sbp("sb_start", [128, NE])
            with ExitStack() as st:
                zt = st.enter_context(nc.sbuf_tensor(uq("sb_zero"), [128, D], BF16))
                S.op("pool", nc.gpsimd.memset, zt[:], 0.0, writes=["zero"])
                for j in range(NBLK_R):
                    S.dma("sp" if j % 2 else "act", self.XS[j * 128:(j + 1) * 128, :], zt[:], reads=["zero"], writes=["dram_XS"])
                S.barrier()
            mstop = os.environ.get("MK_MOE_STOP", "")
            if mstop == "zero":
                return
            with ExitStack() as st:
                sc2 = [self.load_bc(st, "sc2l", self.modd[l, 0, 4 * D:5 * D]), self.load_bc(st, "sc2c", self.modd[l, 1, 4 * D:5 * D])]
                sh2 = [self.load_bc(st, "sh2l", self.modd[l, 0, 3 * D:4 * D]), self.load_bc(st, "sh2c", self.modd[l, 1, 3 * D:4 * D])]
                for (t_, k_) in sc2:
                    S.op("dve", nc.vector.tensor_scalar, t_[:], t_[:], 1.0, None, ALU.add, reads=[k_], writes=[k_])
                wr = st.enter_context(nc.sbuf_tensor(uq("sb_wr"), [128, KC, NE], F32))
                S.dma("sp", wr[:], self.inp["w_router"][l].rearrange("(kc p) e -> p kc e", p=128), writes=["wr"])
                rb = st.enter_context(nc.sbuf_tensor(uq("sb_rb"), [128, NE], F32))
                S.dma("sp", rb[:], self.inp["router_bias"][l, :].partition_broadcast(128), writes=["rb"])
                xp = Pool(nc, st, "rx", [128, D], F32, 2)
                hbp = Pool(nc, st, "rhb", [128, D], BF16, 2)
                hTp = Pool(nc, st, "rhT", [128, KC, 128], F32, 2)
                bip = Pool(nc, st, "rbi", [128, NE], F32, 2)
                v8p = Pool(nc, st, "rv8", [128, 8], F32, 2)
                i8p = Pool(nc, st, "ri8", [128, 8], U32, 2)
                for i in tiles:
                    m = 1 if i < 2 else 0
                    xt, xk = xp.get()
                    S.dma("sp", xt[:], self.xs[i * 128:(i + 1) * 128, :], reads=["dram_xs"], writes=[xk])
                    S.op("dve", nc.vector.tensor_tensor, xt[:], xt[:], sc2[m][0][:], ALU.mult, reads=[xk, sc2[m][1]], writes=[xk])
                    S.op("dve", nc.vector.tensor_tensor, xt[:], xt[:], sh2[m][0][:], ALU.add, reads=[xk, sh2[m][1]], writes=[xk])
                    hb, hbk = hbp.get()
                    S.op("act", nc.scalar.copy, hb[:], xt[:], reads=[xk], writes=[hbk])
                    S.dma("act", self.XS[BASE_SH + i * 128:BASE_SH + (i + 1) * 128, :], hb[:], reads=[hbk], writes=["dram_XS"])
                    hT, hTk = hTp.get()
                    for q in range(4):
                        ps, pk = self.psA.get()
                        for j in range(4):
                            kc = q * 4 + j
                            S.op("pe", nc.tensor.transpose, ps[:, j * 128:(j + 1) * 128], xt[:, kc * 128:(kc + 1) * 128], self.ident[:],
                                 reads=[xk, "ident"], writes=[pk])
                        S.op("act", nc.scalar.copy, hT[:, q * 4:q * 4 + 4, :].rearrange("p k t -> p (k t)"), ps[:, :], reads=[pk], writes=[hTk])
                    ps, pk = self.psB.get()
                    for kc in range(KC):
                        S.op("pe", nc.tensor.matmul, ps[:, :NE], hT[:, kc, :], wr[:, kc, :], start=(kc == 0), stop=(kc == KC - 1),
                             reads=[hTk, "wr"], writes=[pk])
                    S.op("act", nc.scalar.activation, SC[:, i, :], ps[:, :NE], AF.Sigmoid, reads=[pk], writes=["SC"])
                    bi_, bik = bip.get()
                    S.op("dve", nc.vector.tensor_tensor, bi_[:], SC[:, i, :], rb[:], ALU.add, reads=["SC", "rb"], writes=[bik])
                    v8, v8k = v8p.get()
                    S.op("dve", nc.vector.max, v8[:], bi_[:], reads=[bik], writes=[v8k])
                    i8, i8k = i8p.get()
                    S.op("dve", nc.vector.max_index, i8[:], v8[:], bi_[:], reads=[bik, v8k], writes=[i8k])
                    S.op("dve", nc.vector.tensor_copy, IDXF[:, i, :], i8[:], reads=[i8k], writes=["IDXF"])
                    S.op("dve", nc.vector.tensor_scalar, M[:, i, :], bi_[:], v8[:, 7:8], None, ALU.is_ge, reads=[bik, v8k], writes=["M"])
                S.barrier()
            if "MDBG" in self.dbg:
                md = self.dram("MDBG", [128, NT * NE])
                S.dma("sp", md, SC[:].rearrange("p t e -> p (t e)"), reads=["SC"], writes=["dram_MDBG"])
                md2 = self.dram("MDBG2", [128, NT * NE])
                S.dma("sp", md2, M[:].rearrange("p t e -> p (t e)"), reads=["M"], writes=["dram_MDBG2"])
                S.barrier()
            if mstop == "7a":
                return
            with ExitStack() as st:
                def sb(name, shape, dt=F32):
                    return st.enter_context(nc.sbuf_tensor(uq(name), shape, dt))
                sut = sb("sb_sut", [128, 128])
                iota = sb("sb_iota", [128, NE])
                blk = sb("sb_blk", [64, NBIG])
                S.dma("sp", sut[:], self.inp["sut"][:, :], writes=["sut"])
                S.dma("sp", iota[:], self.inp["iota64"][:, :], writes=["iota"])
                S.dma("sp", blk[:], self.inp["blk128"][:, :], writes=["blk"])
                pad = sb("sb_pad", [128, NE])
                padi = sb("sb_padi", [128, NE], I32)
                endb = sb("sb_end", [128, NE])
                padT = sb("sb_padT", [64, 128])
                endT = sb("sb_endT", [64, 128])
                cmp_ = sb("sb_cmp", [64, NBIG])
                bef = sb("sb_bef", [128, NBIG])
                pcol = sb("sb_pcol", [128, 1])
                S.dma("sp", pcol[:], self.inp["pcol"][:, :], writes=["pcol"])
                mcum = sb("sb_mcum", [128, NE])
                ps, pk = self.psA.get()
                for n_, i in enumerate(tiles):
                    S.op("pe", nc.tensor.matmul, ps[:, :NE], self.ones[:], M[:, i, :], start=(n_ == 0), stop=(n_ == ntl - 1),
                         reads=["ones", "M"], writes=[pk])
                S.op("dve", nc.vector.tensor_scalar, pad[:], ps[:, :NE], 1.0 / GRAN, 0.5 - 1.0 / (2 * GRAN), ALU.mult, ALU.add, reads=[pk], writes=["pad"])
                S.op("dve", nc.vector.tensor_copy, padi[:], pad[:], reads=["pad"], writes=["padi"])
                S.op("dve", nc.vector.tensor_copy, pad[:], padi[:], reads=["padi"], writes=["pad"])
                S.op("dve", nc.vector.tensor_scalar, pad[:], pad[:], float(GRAN), None, ALU.mult, reads=["pad"], writes=["pad"])
                ps, pk = self.psA.get()
                S.op("pe", nc.tensor.transpose, ps[:NE, :128], pad[:, :], self.ident[:], reads=["pad", "ident"], writes=[pk])
                S.op("act", nc.scalar.copy, padT[:], ps[:NE, :128], reads=[pk], writes=["padT"])
                ps, pk = self.psA.get()
                S.op("pe", nc.tensor.matmul, ps[:, :NE], padT[:, :], sut[0:NE, 0:NE], start=True, stop=True, reads=["padT", "sut"], writes=[pk])
                S.op("act", nc.scalar.copy, start_bc[:], ps[:, :NE], reads=[pk], writes=["start"])
                S.op("dve", nc.vector.tensor_tensor, endb[:], start_bc[:], pad[:], ALU.add, reads=["start", "pad"], writes=["endb"])
                ps, pk = self.psA.get()
                S.op("pe", nc.tensor.transpose, ps[:NE, :128], endb[:, :], self.ident[:], reads=["endb", "ident"], writes=[pk])
                S.op("act", nc.scalar.copy, endT[:], ps[:NE, :128], reads=[pk], writes=["endT"])
                S.op("dve", nc.vector.tensor_scalar, cmp_[:], blk[:], endT[:, 0:1], None, ALU.is_ge, reads=["blk", "endT"], writes=["cmp"])
                ps, pk = self.psA.get()
                S.op("pe", nc.tensor.matmul, ps[:, :NBIG], self.ones[0:NE, :], cmp_[:, :], start=True, stop=True,
                     reads=["ones", "cmp"], writes=[pk])
                S.op("dve", nc.vector.tensor_scalar, bef[:], ps[:, :NBIG], float(NE - 1), None, ALU.min, reads=[pk], writes=["bef"])
                S.op("dve", nc.vector.tensor_scalar, bef[:], bef[:], 128.0, pcol[:, 0:1], ALU.mult, ALU.add, reads=["bef", "pcol"], writes=["bef"])
                S.op("dve", nc.vector.tensor_scalar, bef[:], bef[:], float(l * NE * 128), None, ALU.add, reads=["bef"], writes=["bef"])
                S.op("dve", nc.vector.tensor_copy, be_i[:], bef[:], reads=["bef"], writes=["be"])
                S.op("pool", nc.gpsimd.memset, mcum[:], 0.0, writes=["mcum"])
                dp = Pool(nc, st, "sdest", [128, NE], F32, 2)
                ohp = Pool(nc, st, "soh", [128, NE], F32, 3)
                jp = Pool(nc, st, "sjunk", [128, NE], F32, 2)
                d8p = Pool(nc, st, "sd8", [128, 8], F32, 2)
                g8p = Pool(nc, st, "sg8", [128, 8], F32, 2)
                hbp = Pool(nc, st, "shb", [128, D], BF16, 3)
                for i in tiles:
                    ps, pk = self.psA.get()
                    S.op("pe", nc.tensor.matmul, ps[:, :NE], sut[:], M[:, i, :], start=True, stop=False, reads=["sut", "M"], writes=[pk])
                    S.op("pe", nc.tensor.matmul, ps[:, :NE], self.ones[:], mcum[:], start=False, stop=True, reads=["ones", "mcum"], writes=[pk])
                    dst, dk = dp.get()
                    S.op("dve", nc.vector.tensor_tensor, dst[:], ps[:, :NE], start_bc[:], ALU.add, reads=[pk, "start"], writes=[dk])
                    S.op("pool", nc.gpsimd.tensor_tensor, mcum[:], mcum[:], M[:, i, :], ALU.add, reads=["mcum", "M"], writes=["mcum"])
                    d8, d8k = d8p.get()
                    g8, g8k = g8p.get()
                    for k in range(8):
                        oh, ohk = ohp.get()
                        S.op("dve", nc.vector.tensor_scalar, oh[:], iota[:], IDXF[:, i, k:k + 1], None, ALU.is_equal,
                             reads=["iota", "IDXF"], writes=[ohk])
                        jk, jkk = jp.get()
                        S.op("dve", nc.vector.scalar_tensor_tensor, jk[:], oh[:], 1.0, dst[:], ALU.mult, ALU.mult, accum_out=d8[:, k:k + 1],
                             reads=[ohk, dk], writes=[jkk, d8k])
                        jk, jkk = jp.get()
                        S.op("dve", nc.vector.scalar_tensor_tensor, jk[:], oh[:], 1.0, SC[:, i, :], ALU.mult, ALU.mult, accum_out=g8[:, k:k + 1],
                             reads=[ohk, "SC"], writes=[jkk, g8k])
                    S.op("dve", nc.vector.tensor_copy, DEST[:, i, :], d8[:], reads=[d8k], writes=["DEST"])
                    sm, smk = self.small.get()
                    S.op("dve", nc.vector.reduce_sum, sm[:, 0:1], g8[:], AX.X, reads=[g8k], writes=[smk])
                    S.op("dve", nc.vector.reciprocal, sm[:, 1:2], sm[:, 0:1], reads=[smk], writes=[smk])
                    S.op("dve", nc.vector.tensor_scalar, GATE[:, i, :], g8[:], sm[:, 1:2], 2.5, ALU.mult, ALU.mult,
                         reads=[g8k, smk], writes=["GATE"])
                    hb, hbk = hbp.get()
                    S.dma("sp", hb[:], self.XS[BASE_SH + i * 128:BASE_SH + (i + 1) * 128, :], reads=["dram_XS"], writes=[hbk])
                    for k in range(8):
                        S.dma("pool", None, None, reads=[hbk, "DEST"], writes=["dram_XS"],
                              indirect=(lambda hb=hb, i=i, k=k: nc.gpsimd.indirect_dma_start(
                                  out=self.XS[:, :], out_offset=bass.IndirectOffsetOnAxis(ap=DEST[:, i, k:k + 1], axis=0),
                                  in_=hb[:, :], in_offset=None)))
                S.barrier()
            if "MDBG3" in self.dbg:
                md3 = self.dram("MDBG3", [128, NT * 8], I32)
                S.dma("sp", md3, DEST[:].rearrange("p t e -> p (t e)"), reads=["DEST"], writes=["dram_MDBG3"])
                md4 = self.dram("MDBG4", [128, NT * 8])
                S.dma("sp", md4, GATE[:].rearrange("p t e -> p (t e)"), reads=["GATE"], writes=["dram_MDBG4"])
                md5 = self.dram("MDBG5", [128, NBIG], I32)
                S.dma("sp", md5, be_i[:], reads=["be"], writes=["dram_MDBG5"])
                S.barrier()
            if mstop == "7c":
                return
            with ExitStack() as st:
                identb = st.enter_context(nc.sbuf_tensor(uq("sb_identb"), [128, 128], BF16))
                S.dma("pool", identb[:], self.inp["ident"][:, :], writes=["identb"])
                w1p = Pool(nc, st, "ew1", [128, KC, EFF], BF16, 2)
                w3p = Pool(nc, st, "ew3", [128, KC, EFF], BF16, 2)
                w2p = Pool(nc, st, "ew2", [128, 4, D], BF16, 2)
                xgp = Pool(nc, st, "exg", [128, D], BF16, 2)
                xTp = Pool(nc, st, "exT", [128, KC, 128], BF16, 2)
                s1p = Pool(nc, st, "es1", [128, EFF], F32, 2)
                gbp = Pool(nc, st, "egb", [128, EFF], BF16, 2)
                gTp = Pool(nc, st, "egT", [128, 4, 128], BF16, 2)
                yp = Pool(nc, st, "ey", [128, D], BF16, 2)

                def ffn_block(row0, w1t, w3t, w2t, wkeys):
                    xg, xgk = xgp.get()
                    S.dma("sp", xg[:], self.XS[row0:row0 + 128, :], reads=["dram_XS"], writes=[xgk])
                    xT, xTk = xTp.get()
                    for hlf in range(2):
                        ps, pk = self.psB.get()
                        pb = ps[:, :].bitcast(BF16)
                        for j in range(8):
                            kc = hlf * 8 + j
                            S.op("pe", nc.tensor.transpose, pb[:, j * 128:(j + 1) * 128], xg[:, kc * 128:(kc + 1) * 128], identb[:],
                                 reads=[xgk, "identb"], writes=[pk])
                        S.op("act", nc.scalar.copy, xT[:, hlf * 8:hlf * 8 + 8, :].rearrange("p k t -> p (k t)"), pb[:, :], reads=[pk], writes=[xTk])
                    p1, p1k = self.psA.get()
                    p3, p3k = self.psA.get()
                    for kc in range(KC):
                        S.op("pe", nc.tensor.matmul, p1[:, :], xT[:, kc, :], w1t[:, kc, :], start=(kc == 0), stop=(kc == KC - 1),
                             reads=[xTk, wkeys[0]], writes=[p1k])
                    for kc in range(KC):
                        S.op("pe", nc.tensor.matmul, p3[:, :], xT[:, kc, :], w3t[:, kc, :], start=(kc == 0), stop=(kc == KC - 1),
                             reads=[xTk, wkeys[1]], writes=[p3k])
                    s1, s1k = s1p.get()
                    S.op("act", nc.scalar.activation, s1[:], p1[:, :], AF.Silu, reads=[p1k], writes=[s1k])
                    gb, gbk = gbp.get()
                    S.op("dve", nc.vector.tensor_tensor, gb[:], p3[:, :], s1[:], ALU.mult, reads=[p3k, s1k], writes=[gbk])
                    ps, pk = self.psB.get()
                    pb = ps[:, :].bitcast(BF16)
                    for fc in range(4):
                        S.op("pe", nc.tensor.transpose, pb[:, fc * 128:(fc + 1) * 128], gb[:, fc * 128:(fc + 1) * 128], identb[:],
                             reads=[gbk, "identb"], writes=[pk])
                    gT, gTk = gTp.get()
                    S.op("act", nc.scalar.copy, gT[:].rearrange("p k t -> p (k t)"), pb[:, :512], reads=[pk], writes=[gTk])
                    y, yk = yp.get()
                    for q in range(4):
                        ps, pk = self.psA.get()
                        for fc in range(4):
                            S.op("pe", nc.tensor.matmul, ps[:, :], gT[:, fc, :], w2t[:, fc, q * 512:(q + 1) * 512], start=(fc == 0), stop=(fc == 3),
                                 reads=[gTk, wkeys[2]], writes=[pk])
                        if q % 2 == 0:
                            S.op("dve", nc.vector.tensor_copy, y[:, q * 512:(q + 1) * 512], ps[:, :], reads=[pk], writes=[yk])
                        else:
                            S.op("act", nc.scalar.copy, y[:, q * 512:(q + 1) * 512], ps[:, :], reads=[pk], writes=[yk])
                    S.dma("act", self.YS[row0:row0 + 128, :], y[:], reads=[yk], writes=["dram_YS"])

                nblk = int(os.environ.get("MK_MOE_NBLK", str(NBIG)))
                for j in range(nblk):
                    w1t, w1k = w1p.get()
                    w3t, w3k = w3p.get()
                    w2t, w2k = w2p.get()
                    for (wt_, wk_, nm) in ((w1t, w1k, "w1r"), (w3t, w3k, "w3r"), (w2t, w2k, "w2r")):
                        oap = wt_[:].rearrange("p a n -> p (a n)")
                        S.dma("pool", None, None, reads=["be"], writes=[wk_],
                              indirect=(lambda oap=oap, nm=nm, j=j: nc.gpsimd.indirect_dma_start(
                                  out=oap, out_offset=None, in_=self.inp[nm][:, :],
                                  in_offset=bass.IndirectOffsetOnAxis(ap=be_i[:, j:j + 1], axis=0))))
                    for sub in range(GRAN // 128):
                        ffn_block(j * GRAN + sub * 128, w1t, w3t, w2t, (w1k, w3k, w2k))
                w1t, w1k = w1p.get()
                w3t, w3k = w3p.get()
                w2t, w2k = w2p.get()
                S.dma("pool", w1t[:], self.inp["ws1"][l].rearrange("(kc p) n -> p kc n", p=128), writes=[w1k])
                S.dma("pool", w3t[:], self.inp["ws3"][l].rearrange("(kc p) n -> p kc n", p=128), writes=[w3k])
                S.dma("pool", w2t[:], self.inp["ws2"][l].rearrange("(fc p) n -> p fc n", p=128), writes=[w2k])
                for i in tiles:
                    ffn_block(BASE_SH + i * 128, w1t, w3t, w2t, (w1k, w3k, w2k))
                S.barrier()
            if mstop == "7d":
                return
            with ExitStack() as st:
                gt = [self.load_bc(st, "gt2l", self.modd[l, 0, 5 * D:6 * D]), self.load_bc(st, "gt2c", self.modd[l, 1, 5 * D:6 * D])]
                lng, lngk = self.load_bc(st, "ln2g", self.inp["ln2_g"][l, :])
                lnb, lnbk = self.load_bc(st, "ln2b", self.inp["ln2_b"][l, :])
                accp = Pool(nc, st, "cacc", [128, D], F32, 2)
                a0p = Pool(nc, st, "ca0", [128, D], BF16, 2)
                ykp = Pool(nc, st, "cyk", [128, D], BF16, 3)
                xp = Pool(nc, st, "cx", [128, D], F32, 2)
                junk = Pool(nc, st, "cjunk", [128, D], F32, 1)
                for i in tiles:
                    m = 1 if i < 2 else 0
                    acc, acck = accp.get()
                    a0, a0k = a0p.get()
                    S.dma("sp", a0[:], self.YS[BASE_SH + i * 128:BASE_SH + (i + 1) * 128, :], reads=["dram_YS"], writes=[a0k])
                    S.op("act", nc.scalar.copy, acc[:], a0[:], reads=[a0k], writes=[acck])
                    for k in range(8):
                        yk_, ykk = ykp.get()
                        S.dma("pool", None, None, reads=["dram_YS", "DEST"], writes=[ykk],
                              indirect=(lambda yk_=yk_, i=i, k=k: nc.gpsimd.indirect_dma_start(
                                  out=yk_[:, :], out_offset=None, in_=self.YS[:, :],
                                  in_offset=bass.IndirectOffsetOnAxis(ap=DEST[:, i, k:k + 1], axis=0))))
                        S.op("dve", nc.vector.scalar_tensor_tensor, acc[:], yk_[:], GATE[:, i, k:k + 1], acc[:], ALU.mult, ALU.add,
                             reads=[ykk, "GATE", acck], writes=[acck])
                    xt, xk = xp.get()
                    S.dma("act", xt[:], self.xs[i * 128:(i + 1) * 128, :], reads=["dram_xs"], writes=[xk])
                    S.op("dve", nc.vector.tensor_tensor, acc[:], acc[:], gt[m][0][:], ALU.mult, reads=[acck, gt[m][1]], writes=[acck])
                    S.op("dve", nc.vector.scalar_tensor_tensor, acc[:], xt[:], ALPHA, acc[:], ALU.mult, ALU.add,
                         reads=[xk, acck], writes=[acck])
                    self.ln_rows(S, acc, acck, self.small, gb=(lng, lnb, lngk, lnbk), junk=junk)
                    if last:
                        S.dma("sp", self.out[(i - 2) * 128:(i - 1) * 128, :], acc[:], reads=[acck], writes=["dram_out"])
                    else:
                        S.dma("sp", self.xs[i * 128:(i + 1) * 128, :], acc[:], reads=[acck], writes=["dram_xs"])
                S.barrier()


_CACHE = {}


def _layout_inputs(inputs, core):
    m = {}
    x = np.asarray(inputs["x"], dtype=np.float32)
    ctx = np.asarray(inputs["ctx"], dtype=np.float32)
    c = np.asarray(inputs["c"], dtype=np.float32)
    cc = np.asarray(inputs["c_ctx"], dtype=np.float32)
    xs, cs = [], []
    for j in range(NB):
        b = (core * NB + j) % 4
        xs.append(np.concatenate([ctx[b], x[b]], axis=0))
        cs.append(np.stack([c[b].reshape(KC, 128).T, cc.reshape(KC, 128).T], axis=-1))
    m["xin"] = np.ascontiguousarray(np.stack(xs, axis=0))
    m["cT"] = np.ascontiguousarray(np.stack(cs, axis=0))
    return m


def kernel(**inputs):
    dbg = tuple(x for x in os.environ.get("MK_DBG", "").split(",") if x)
    stop = os.environ.get("MK_STOP", "all")
    ncores = int(os.environ.get("MK_CORES", str(4 // NB)))
    bld = Builder(dbg)
    nc = bld.build(stop)
    consts = host_consts()
    shared = {}
    for k in bld.inp.used:
        if k not in W_SHAPES:
            continue
        if k in ("w1r", "w3r"):
            w = np.asarray(inputs["w1"] if k == "w1r" else inputs["w3"], dtype=np.float32)
            w = w.reshape(DEPTH, NE, KC, 128, EFF).transpose(0, 1, 3, 2, 4)
            shared[k] = np.ascontiguousarray(w).reshape(DEPTH * NE * 128, 8192)
        elif k == "w2r":
            w = np.asarray(inputs["w2"], dtype=np.float32)
            w = w.reshape(DEPTH, NE, 4, 128, D).transpose(0, 1, 3, 2, 4)
            shared[k] = np.ascontiguousarray(w).reshape(DEPTH * NE * 128, 8192)
        else:
            shared[k] = np.ascontiguousarray(np.asarray(inputs[k], dtype=np.float32))
    consts = {k: v for k, v in consts.items() if k in bld.inp.used}
    in_maps = []
    for core in range(ncores):
        m = _layout_inputs(inputs, core)
        m.update(consts)
        m.update(shared)
        in_maps.append(m)
    res = run_bass_kernel_spmd(nc, in_maps, core_ids=list(range(ncores)))
    kernel.last = res
    out = np.concatenate([np.asarray(res.results[c]["out"]) for c in range(ncores)], axis=0)
    return np.ascontiguousarray(out[:4]).astype(np.float32)
```
